# Optimizing a Trainium2 kernel written in Bass

```python
import jax, jax.numpy as jnp
from jax import lax
import numpy as np

D_MODEL = 1024
BATCH = 8
SEQ = 8192
DEPTH = 2

A_HEADS = 4
A_HEAD_DIM = 128
D_A = A_HEADS * A_HEAD_DIM
CHUNK = 128
B_GROUPS = 4
D_B = D_MODEL - D_A
B_CONV = 3
D_IN_AB = 2 * D_A + 3 * D_B
D_RNN = D_MODEL
LRU_HEADS = 8
LRU_HEAD_DIM = D_RNN // LRU_HEADS
C_CONV = 4
LRU_C = 8.0
N_GROUPS = 4
EXPERTS_PER_GROUP = 8
N_EXPERTS = N_GROUPS * EXPERTS_PER_GROUP
TOP_K = 2
D_EXPERT = 512
MOE_BLOCK = 128
N_EVEN = (DEPTH + 1) // 2
N_ODD = DEPTH // 2
EPS = 1e-6

kernel_name = "hybrid_gmlp_shortconv_rglru_hmoe"


def rms_norm(x, g):
    xf = x.astype(jnp.float32)
    y = xf * lax.rsqrt(jnp.mean(jnp.square(xf), axis=-1, keepdims=True) + EPS)
    return y.astype(x.dtype) * g


def layer_norm(x, g):
    xf = x.astype(jnp.float32)
    mu = jnp.mean(xf, axis=-1, keepdims=True)
    var = jnp.mean(jnp.square(xf - mu), axis=-1, keepdims=True)
    return ((xf - mu) * lax.rsqrt(var + EPS)).astype(x.dtype) * g


def causal_conv(x, w):
    k_width = w.shape[0]
    t_len = x.shape[1]
    xp = jnp.pad(x, ((0, 0), (k_width - 1, 0), (0, 0)))
    y = xp[:, 0:t_len] * w[0]
    for k in range(1, k_width):
        y = y + xp[:, k:k + t_len] * w[k]
    return y


def mixer_ab(h, w_in, a_ln_g, a_ws, a_ws_b, b_conv_w, w_out):
    bsz, t_len, _ = h.shape
    z = h @ w_in
    u, v, gate_b, gate_c, xb = jnp.split(
        z, [D_A, 2 * D_A, 2 * D_A + D_B, 2 * D_A + 2 * D_B], axis=-1)
    u = jax.nn.gelu(u)
    v = layer_norm(jax.nn.gelu(v), a_ln_g)
    vc = v.reshape(bsz, t_len // CHUNK, CHUNK, A_HEADS, A_HEAD_DIM)
    causal = jnp.tril(jnp.ones((CHUNK, CHUNK), dtype=bool))
    ws = jnp.where(causal, a_ws, 0.0)
    mixed = jnp.einsum('hts,bcshd->bcthd', ws, vc) + a_ws_b.T[:, :, None]
    y_a = u * mixed.reshape(bsz, t_len, D_A)
    y_b = gate_b * causal_conv(gate_c * xb, b_conv_w)
    return jnp.concatenate([y_a, y_b], axis=-1) @ w_out


def mixer_rglru(h, w_in, conv_w, conv_b, w_a, b_a, w_x, b_x, lam, w_out):
    bsz, t_len, _ = h.shape
    gate, xr = jnp.split(h @ w_in, [D_RNN], axis=-1)
    gate = jax.nn.gelu(gate)
    xr = causal_conv(xr, conv_w) + conv_b
    xh = xr.reshape(bsz, t_len, LRU_HEADS, LRU_HEAD_DIM)
    r = jax.nn.sigmoid(jnp.einsum('bthi,hij->bthj', xh, w_a).reshape(bsz, t_len, D_RNN) + b_a)
    i = jax.nn.sigmoid(jnp.einsum('bthi,hij->bthj', xh, w_x).reshape(bsz, t_len, D_RNN) + b_x)
    log_a = (LRU_C * r.astype(jnp.float32)) * jax.nn.log_sigmoid(lam.astype(jnp.float32))
    a = jnp.exp(log_a)
    b = jnp.sqrt(-jnp.expm1(2.0 * log_a)) * (i * xr).astype(jnp.float32)

    def combine(left, right):
        a_l, b_l = left
        a_r, b_r = right
        return a_l * a_r, a_r * b_l + b_r

    _, hseq = lax.associative_scan(combine, (a, b), axis=1)
    return (gate * hseq.astype(h.dtype)) @ w_out


def hierarchical_moe(h, w_rg, b_rg, w_re, b_re, w_gate, w_up, w_down):
    bsz, t_len, d = h.shape
    n_tok = bsz * t_len
    xt = h.reshape(n_tok, d)
    g_logits = (xt @ w_rg + b_rg).astype(jnp.float32)
    g_idx = jnp.argmax(g_logits, axis=-1)
    g_prob = jnp.take_along_axis(jax.nn.softmax(g_logits, axis=-1), g_idx[:, None], axis=-1)
    e_logits = (xt @ w_re + b_re).astype(jnp.float32).reshape(n_tok, N_GROUPS, EXPERTS_PER_GROUP)
    e_sel = jnp.take_along_axis(e_logits, g_idx[:, None, None], axis=1)[:, 0]
    top_val, top_idx = lax.top_k(e_sel, TOP_K)
    gates = g_prob * jax.nn.softmax(top_val, axis=-1)
    expert_ids = g_idx[:, None].astype(jnp.int32) * EXPERTS_PER_GROUP + top_idx.astype(jnp.int32)

    n_slots = n_tok * TOP_K
    n_blocks = -(-n_slots // MOE_BLOCK) + N_EXPERTS
    flat_e = expert_ids.reshape(n_slots)
    flat_tok = jnp.repeat(jnp.arange(n_tok, dtype=jnp.int32), TOP_K)
    flat_g = gates.reshape(n_slots)
    order = jnp.argsort(flat_e)
    sorted_e = flat_e[order]
    counts = jnp.bincount(flat_e, length=N_EXPERTS)
    starts = jnp.cumsum(counts) - counts
    padded = (counts + MOE_BLOCK - 1) // MOE_BLOCK * MOE_BLOCK
    pad_ends = jnp.cumsum(padded)
    pad_starts = pad_ends - padded
    dest = pad_starts[sorted_e] + (jnp.arange(n_slots, dtype=jnp.int32) - starts[sorted_e])
    slot_tok = jnp.full((n_blocks * MOE_BLOCK,), n_tok, jnp.int32).at[dest].set(flat_tok[order])
    slot_g = jnp.zeros((n_blocks * MOE_BLOCK,), jnp.float32).at[dest].set(flat_g[order])
    block_e = jnp.minimum(
        jnp.searchsorted(pad_ends, jnp.arange(n_blocks, dtype=jnp.int32) * MOE_BLOCK, side='right'),
        N_EXPERTS - 1)
    x_pad = jnp.concatenate([xt, jnp.zeros((1, d), xt.dtype)], axis=0)

    def expert_block(args):
        tok_b, g_b, e = args
        xb = x_pad[tok_b]
        hid = jax.nn.silu(xb @ w_gate[e]) * (xb @ w_up[e])
        return (hid @ w_down[e]) * g_b[:, None].astype(xb.dtype)

    y = lax.map(expert_block, (slot_tok.reshape(n_blocks, MOE_BLOCK),
                               slot_g.reshape(n_blocks, MOE_BLOCK), block_e))
    out = jax.ops.segment_sum(y.reshape(-1, d), slot_tok, num_segments=n_tok + 1)[:n_tok]
    return out.reshape(bsz, t_len, d)


def setup_inputs(seed: int = 0) -> dict:
    key = jax.random.key(seed)
    ks = jax.random.split(key, 26)

    def nrm(k, shape, scale):
        return jax.random.normal(k, shape, jnp.float32) * scale

    a8 = jax.random.uniform(ks[17], (N_ODD, D_RNN), jnp.float32, 0.9, 0.999)
    a_base = a8 ** (1.0 / LRU_C)
    c_lambda = jnp.log(a_base) - jnp.log1p(-a_base)
    return {
        "x": nrm(ks[0], (BATCH, SEQ, D_MODEL), 1.0),
        "norm_mix_g": 1.0 + nrm(ks[1], (DEPTH, D_MODEL), 0.05),
        "norm_ffn_g": 1.0 + nrm(ks[2], (DEPTH, D_MODEL), 0.05),
        "norm_final_g": 1.0 + nrm(ks[3], (D_MODEL,), 0.05),
        "ab_w_in": nrm(ks[4], (N_EVEN, D_MODEL, D_IN_AB), D_MODEL ** -0.5),
        "a_ln_g": 1.0 + nrm(ks[5], (N_EVEN, D_A), 0.05),
        "a_ws": nrm(ks[6], (N_EVEN, A_HEADS, CHUNK, CHUNK), CHUNK ** -0.5),
        "a_ws_b": 1.0 + nrm(ks[7], (N_EVEN, A_HEADS, CHUNK), 0.1),
        "b_conv_w": nrm(ks[8], (N_EVEN, B_CONV, D_B), B_CONV ** -0.5),
        "ab_w_out": nrm(ks[9], (N_EVEN, D_A + D_B, D_MODEL), (D_A + D_B) ** -0.5),
        "c_w_in": nrm(ks[10], (N_ODD, D_MODEL, 2 * D_RNN), D_MODEL ** -0.5),
        "c_conv_w": nrm(ks[11], (N_ODD, C_CONV, D_RNN), C_CONV ** -0.5),
        "c_conv_b": nrm(ks[12], (N_ODD, D_RNN), 0.01),
        "c_w_a": nrm(ks[13], (N_ODD, LRU_HEADS, LRU_HEAD_DIM, LRU_HEAD_DIM), LRU_HEAD_DIM ** -0.5),
        "c_b_a": nrm(ks[14], (N_ODD, D_RNN), 0.01),
        "c_w_x": nrm(ks[15], (N_ODD, LRU_HEADS, LRU_HEAD_DIM, LRU_HEAD_DIM), LRU_HEAD_DIM ** -0.5),
        "c_b_x": nrm(ks[16], (N_ODD, D_RNN), 0.01),
        "c_lambda": c_lambda,
        "c_w_out": nrm(ks[18], (N_ODD, D_RNN, D_MODEL), D_RNN ** -0.5),
        "moe_w_rg": nrm(ks[19], (DEPTH, D_MODEL, N_GROUPS), D_MODEL ** -0.5),
        "moe_b_rg": nrm(ks[20], (DEPTH, N_GROUPS), 0.01),
        "moe_w_re": nrm(ks[21], (DEPTH, D_MODEL, N_EXPERTS), D_MODEL ** -0.5),
        "moe_b_re": nrm(ks[22], (DEPTH, N_EXPERTS), 0.01),
        "moe_w_gate": nrm(ks[23], (DEPTH, N_EXPERTS, D_MODEL, D_EXPERT), D_MODEL ** -0.5),
        "moe_w_up": nrm(ks[24], (DEPTH, N_EXPERTS, D_MODEL, D_EXPERT), D_MODEL ** -0.5),
        "moe_w_down": nrm(ks[25], (DEPTH, N_EXPERTS, D_EXPERT, D_MODEL), D_EXPERT ** -0.5),
    }


def reference(x, norm_mix_g, norm_ffn_g, norm_final_g,
              ab_w_in, a_ln_g, a_ws, a_ws_b, b_conv_w, ab_w_out,
              c_w_in, c_conv_w, c_conv_b, c_w_a, c_b_a, c_w_x, c_b_x, c_lambda, c_w_out,
              moe_w_rg, moe_b_rg, moe_w_re, moe_b_re, moe_w_gate, moe_w_up, moe_w_down):
    for layer in range(DEPTH):
        idx = layer // 2
        h = rms_norm(x, norm_mix_g[layer])
        if layer % 2 == 0:
            x = x + mixer_ab(h, ab_w_in[idx], a_ln_g[idx], a_ws[idx], a_ws_b[idx],
                             b_conv_w[idx], ab_w_out[idx])
        else:
            x = x + mixer_rglru(h, c_w_in[idx], c_conv_w[idx], c_conv_b[idx], c_w_a[idx],
                                c_b_a[idx], c_w_x[idx], c_b_x[idx], c_lambda[idx], c_w_out[idx])
        h = rms_norm(x, norm_ffn_g[layer])
        x = x + hierarchical_moe(h, moe_w_rg[layer], moe_b_rg[layer], moe_w_re[layer],
                                 moe_b_re[layer], moe_w_gate[layer], moe_w_up[layer],
                                 moe_w_down[layer])
    return rms_norm(x, norm_final_g)
```

```python
import numpy as np
from contextlib import ExitStack
import concourse.bass as bass
import concourse.mybir as mybir
from concourse.bass_utils import run_bass_kernel_spmd

F32 = mybir.dt.float32
BF16 = mybir.dt.bfloat16
I32 = mybir.dt.int32
AF = mybir.ActivationFunctionType
ALU = mybir.AluOpType
AX = mybir.AxisListType

SETTLE = False
SERIAL_IND = False
P = 128
D = 1024
KD = 8
NE = 32
EPS = 1e-6


class Sem:
    def __init__(self, h, owner=None):
        self.h = h
        self.val = 0
        self.owner = owner


class Buf:
    def __init__(self, name):
        self.name = name
        self.w = {}
        self.r = {}


class T:
    def __init__(self, t, name):
        self.t = t
        self.b = Buf(name)


class Sch:
    def __init__(self, nc, es):
        self.nc = nc
        self.es = es
        self.engs = {"pe": nc.tensor, "act": nc.scalar, "dve": nc.vector, "pool": nc.gpsimd, "sp": nc.sync}
        self.esem = {}
        self.known = {e: {} for e in self.engs}
        self.dsems = []
        self.nsem = 0
        for e in self.engs:
            self.esem[e] = self.newsem(e)
        self.scratch = {e: es.enter_context(nc.sbuf_tensor(f"scr_{e}", [P, 2], F32)) for e in ("act", "dve", "pool")}
        nc.vector.memset(self.scratch["act"][:], 0.0)

    def newsem(self, owner=None):
        self.nsem += 1
        h = self.es.enter_context(self.nc.semaphore(f"s{self.nsem}"))
        s = Sem(h, owner)
        if owner is None:
            self.dsems.append(s)
        return s

    def _waits(self, eng, reads, writes):
        need = {}
        for b in reads:
            for s, v in b.w.items():
                if need.get(s, 0) < v:
                    need[s] = v
        for b in writes:
            for dct in (b.w, b.r):
                for s, v in dct.items():
                    if need.get(s, 0) < v:
                        need[s] = v
        E = self.engs[eng]
        kn = self.known[eng]
        for s, v in need.items():
            if s.owner == eng and eng == "pe":
                continue
            if SETTLE and s.owner in ("act", "dve", "pool") and s.owner != eng:
                v = v + 1
                if s.val < v:
                    self._dummy(s.owner)
            if kn.get(s, 0) >= v:
                continue
            E.wait_ge(s.h, v)
            kn[s] = v

    def _dummy(self, eng):
        E = self.engs[eng]
        t = self.scratch[eng]
        if eng == "act":
            ins = E.activation(out=t[:, 1:2], in_=t[:, 0:1], func=AF.Copy)
        else:
            ins = E.memset(t[:, 0:1], 0.0)
        s = self.esem[eng]
        s.val += 1
        ins.then_inc(s.h, 1)

    def _mark(self, s, v, reads, writes):
        for b in reads:
            if b.r.get(s, 0) < v:
                b.r[s] = v
        for b in writes:
            if b.w.get(s, 0) < v:
                b.w[s] = v

    def op(self, eng, fn, r=(), w=()):
        reads, writes = r, w
        reads = [x.b if isinstance(x, T) else x for x in reads]
        writes = [x.b if isinstance(x, T) else x for x in writes]
        self._waits(eng, reads, writes)
        ins = fn(self.engs[eng])
        s = self.esem[eng]
        s.val += 1
        ins.then_inc(s.h, 1)
        self._mark(s, s.val, reads, writes)

    def dma(self, q, sem, fn, r=(), w=(), serial=False, defer=None):
        reads, writes = r, w
        reads = [x.b if isinstance(x, T) else x for x in reads]
        writes = [x.b if isinstance(x, T) else x for x in writes]
        self._waits(q, reads, writes)
        if serial and SERIAL_IND:
            last = getattr(self, "last_ind", None)
            if last is not None and self.known[q].get(last[0], 0) < last[1]:
                self.engs[q].wait_ge(last[0].h, last[1])
                self.known[q][last[0]] = last[1]
        ins = fn(self.engs[q])
        sem.val += 16
        ins.then_inc(sem.h, 16)
        if serial:
            self.last_ind = (sem, sem.val)
        if defer is not None:
            defer.append((sem, writes))
            self._mark(sem, sem.val, reads, [])
        else:
            self._mark(sem, sem.val, reads, writes)

    def flush(self, lst):
        for sem, writes in lst:
            self._mark(sem, sem.val, [], writes)
        del lst[:]

    def barrier(self):
        allsems = list(self.esem.values()) + self.dsems
        for e, E in self.engs.items():
            kn = self.known[e]
            for s in allsems:
                if s.owner == e or s.val == 0:
                    continue
                if kn.get(s, 0) >= s.val:
                    continue
                E.wait_ge(s.h, s.val)
                kn[s] = s.val


def interleave(*gens):
    gens = [g for g in gens if g is not None]
    while gens:
        for g in list(gens):
            try:
                next(g)
            except StopIteration:
                gens.remove(g)


def build(N, CAP, stop_after=5, MT1=512, MT3=256, debug=False):
    NT = N // P
    NSLOT = NE * CAP
    nc = bass.Bass("TRN2", target_bir_lowering=False)

    def din(name, shape, dt=F32):
        return nc.dram_tensor(name, list(shape), dt, kind="ExternalInput").ap()

    x_d = din("x", [N, D])
    norm_mix_g = din("norm_mix_g", [2, D])
    norm_ffn_g = din("norm_ffn_g", [2, D])
    norm_final_g = din("norm_final_g", [D])
    ab_w_in = din("ab_w_in", [1, D, 2560])
    a_ln_g = din("a_ln_g", [1, 512])
    a_ws = din("a_ws", [1, 4, 128, 128])
    a_ws_b = din("a_ws_b", [1, 4, 128])
    b_conv_w = din("b_conv_w", [1, 3, 512])
    ab_w_out = din("ab_w_out", [1, D, D])
    c_w_in = din("c_w_in", [1, D, 2048])
    c_conv_w = din("c_conv_w", [1, 4, D])
    c_conv_b = din("c_conv_b", [1, D])
    c_w_a = din("c_w_a", [1, 8, 128, 128])
    c_b_a = din("c_b_a", [1, D])
    c_w_x = din("c_w_x", [1, 8, 128, 128])
    c_b_x = din("c_b_x", [1, D])
    c_lambda = din("c_lambda", [1, D])
    c_w_out = din("c_w_out", [1, D, D])
    moe_w_rg = din("moe_w_rg", [2, D, 4])
    moe_b_rg = din("moe_b_rg", [2, 4])
    moe_w_re = din("moe_w_re", [2, D, 32])
    moe_b_re = din("moe_b_re", [2, 32])
    moe_w_gate = din("moe_w_gate", [2, NE, D, 512])
    moe_w_up = din("moe_w_up", [2, NE, D, 512])
    moe_w_down = din("moe_w_down", [2, NE, 512, D])
    out_d = nc.dram_tensor("out", [N, D], F32, kind="ExternalOutput").ap()
    dbg_d = nc.dram_tensor("dbg", [P, NT * 4], F32, kind="ExternalOutput").ap()
    dbg2_d = nc.dram_tensor("dbg2", [P, 16384], F32, kind="ExternalOutput").ap() if debug else None
    dumps = {}
    xres_d = nc.dram_tensor("xres", [N, D], F32, kind="Internal").ap()
    xs_d = nc.dram_tensor("xs", [NSLOT, D], BF16, kind="Internal").ap()
    ys_d = nc.dram_tensor("ys", [NSLOT, D], BF16, kind="Internal").ap()
    xres_b = Buf("xres")
    xs_b = Buf("xs")
    xs_zero = Buf("xs_zero")
    ys_b = Buf("ys")

    with ExitStack() as es:
        S = Sch(nc, es)

        sbn = [0]

        def sb(name, shape, dt, scope=es):
            sbn[0] += 1
            name = f"{name}_{sbn[0]}"
            return T(scope.enter_context(nc.sbuf_tensor(name, list(shape), dt)), name)

        def act(fn, r=(), w=()):
            S.op("act", fn, r, w)

        def dve(fn, r=(), w=()):
            S.op("dve", fn, r, w)

        def pool(fn, r=(), w=()):
            S.op("pool", fn, r, w)

        def pe(fn, r=(), w=()):
            S.op("pe", fn, r, w)

        dcol = [0]
        curscope = [es]

        def dump(name, ap, width, rd):
            if not debug or name in dumps:
                return
            stt = sb("dmp_" + name, [P, width], F32, curscope[0])
            dsem = S.newsem()
            dve(lambda e: e.tensor_copy(stt.t[0:ap.shape[0], :], ap), r=rd, w=[stt])
            c0 = dcol[0]
            S.dma("sp", dsem, lambda e: e.dma_start(out=dbg2_d[0:ap.shape[0], c0:c0 + width], in_=stt.t[0:ap.shape[0], :]), r=[stt], w=[])
            dumps[name] = (c0, width)
            dcol[0] += width

        ps = [T(es.enter_context(nc.psum_tensor(f"ps{i}", [P, 512], F32)), f"ps{i}") for i in range(6)]
        psO = T(es.enter_context(nc.psum_tensor("psO", [P, 1024], F32)), "psO")

        iot = sb("iot", [P, P], I32)
        ident_f = sb("ident_f", [P, P], F32)
        ident_b = sb("ident_b", [P, P], BF16)
        tri_f = sb("tri_f", [P, P], F32)
        ones_row_f = sb("ones_row_f", [1, P], F32)
        ones_row_b = sb("ones_row_b", [1, P], BF16)
        ones_col_f = sb("ones_col_f", [P, 1], F32)
        econst = sb("econst", [P, 16], F32)
        ones_mat = sb("ones_mat", [P, P], F32)
        ebaseN = sb("ebaseN", [P, 4 * NE], F32)
        mhalf = sb("mhalf", [P, 8], F32)
        ebase_i = sb("ebase_i", [P, NE], I32)
        ebase = sb("ebase", [P, NE], F32)
        base_row = sb("base_row", [1, NE], F32)
        tokinfo = sb("tokinfo", [P, NT, 4], F32)
        tokidx = sb("tokidx", [P, NT, 2], I32)
        gfin_rep = sb("gfin_rep", [P, D], F32)
        csem = S.newsem()

        pool(lambda e: e.iota(iot.t[:], pattern=[[1, P]], base=0, channel_multiplier=-1), w=[iot])
        dve(lambda e: e.tensor_scalar(ident_f.t[:], iot.t[:], 0.0, None, op0=ALU.is_equal), r=[iot], w=[ident_f])
        dve(lambda e: e.tensor_scalar(tri_f.t[:], iot.t[:], 0.0, None, op0=ALU.is_ge), r=[iot], w=[tri_f])
        dve(lambda e: e.tensor_copy(ident_b.t[:], ident_f.t[:]), r=[ident_f], w=[ident_b])
        pool(lambda e: e.memset(ones_row_f.t[:], 1.0), w=[ones_row_f])
        pool(lambda e: e.memset(ones_row_b.t[:], 1.0), w=[ones_row_b])
        pool(lambda e: e.memset(ones_col_f.t[:], 1.0), w=[ones_col_f])
        pool(lambda e: e.memset(econst.t[:], float(np.e)), w=[econst])
        pool(lambda e: e.memset(mhalf.t[:], -0.5), w=[mhalf])
        pool(lambda e: e.iota(ebase_i.t[:], pattern=[[CAP, NE]], base=0, channel_multiplier=0), w=[ebase_i])
        dve(lambda e: e.tensor_copy(ebase.t[:], ebase_i.t[:]), r=[ebase_i], w=[ebase])
        for j_ in range(4):
            dve(lambda e, j_=j_: e.tensor_copy(ebaseN.t[:, j_ * NE:(j_ + 1) * NE], ebase_i.t[:]), r=[ebase_i], w=[ebaseN])
        dve(lambda e: e.memset(ones_mat.t[:], 1.0), w=[ones_mat])
        S.dma("sp", csem, lambda e: e.dma_start(out=gfin_rep.t[:], in_=norm_final_g.partition_broadcast(P)), w=[gfin_rep])

        bound_reg = nc.gpsimd.to_reg(NSLOT - 1)

        def rstd_from_ss(rstd, ss, n, cols):
            pool(lambda e: e.tensor_scalar(rstd.t[:, 0:cols], ss.t[:, 0:cols], 1.0 / n, EPS, op0=ALU.mult, op1=ALU.add),
                 r=[ss], w=[rstd])
            pool(lambda e: e.tensor_tensor(rstd.t[:, 0:cols], rstd.t[:, 0:cols], mhalf.t[:, 0:cols], op=ALU.pow),
                 r=[rstd, mhalf], w=[rstd])

        def load_cast_rows(scope_stg, stgsems, dst, dram_rows_fn, nk, width, gcol=None, cnt=[0]):
            for kc in range(nk):
                i = cnt[0] % 2
                cnt[0] += 1
                st = scope_stg[i]
                S.dma("sp", stgsems[i], lambda e, kc=kc, st=st: e.dma_start(out=st.t[:, 0:width], in_=dram_rows_fn(kc)), w=[st])
                if gcol is not None:
                    if kc % 2 == 0:
                        dve(lambda e, kc=kc, st=st: e.tensor_scalar(dst.t[:, kc, :], st.t[:, 0:width], gcol.t[:, kc:kc + 1], None, op0=ALU.mult),
                            r=[st, gcol], w=[dst])
                    else:
                        act(lambda e, kc=kc, st=st: e.activation(out=dst.t[:, kc, :], in_=st.t[:, 0:width], func=AF.Identity, scale=gcol.t[:, kc:kc + 1]),
                            r=[st, gcol], w=[dst])
                else:
                    if kc % 2 == 0:
                        dve(lambda e, kc=kc, st=st: e.tensor_copy(dst.t[:, kc, :], st.t[:, 0:width]), r=[st], w=[dst])
                    else:
                        act(lambda e, kc=kc, st=st: e.activation(out=dst.t[:, kc, :], in_=st.t[:, 0:width], func=AF.Copy), r=[st], w=[dst])

        def norm_and_transpose(sc, xt, nj, hb_l, hT, ss, rstd, junk, PT):
            for j in range(nj):
                act(lambda e, j=j: e.activation(out=junk.t[:], in_=xt.t[:, j, :], func=AF.Square, accum_out=ss.t[:, j:j + 1]),
                    r=[xt], w=[junk, ss])
            rstd_from_ss(rstd, ss, D, nj)
            dump("ss", ss.t[:, 0:nj], nj, [ss])
            dump("rstd", rstd.t[:, 0:nj], nj, [rstd])
            for j in range(nj):
                hb = hb_l[j % 2]
                dve(lambda e, j=j, hb=hb: e.tensor_scalar(hb.t[:], xt.t[:, j, :], rstd.t[:, j:j + 1], None, op0=ALU.mult),
                    r=[xt, rstd], w=[hb])
                pass
                ptv = PT.t[:].bitcast(BF16)
                for kc in range(KD):
                    pe(lambda e, kc=kc, hb=hb: e.transpose(ptv[:, kc * P:(kc + 1) * P], hb.t[:, kc * P:(kc + 1) * P], ident_b.t[:]),
                       r=[hb, ident_b], w=[PT])
                act(lambda e, j=j: e.activation(out=hT.t[:, :, j * P:(j + 1) * P], in_=ptv.rearrange("p (k t) -> p k t", k=KD), func=AF.Copy),
                    r=[PT], w=[hT])
                dump("hT0", hT.t[:, 0, 0:P], P, [hT])

        def ffn_norm_route_scatter(sc, lyr, xt, j, tile, W):
            (gffn_rep, wr, br_row, ss2, rstd2, junk, h2g_l, h2b_l, h2T, PSR, lg, rt, scat_sems) = W
            h2g = h2g_l[tile % 2]
            h2b = h2b_l[tile % 2]
            act(lambda e: e.activation(out=junk.t[:], in_=xt.t[:, j, :], func=AF.Square, accum_out=ss2.t[:, 0:1]), r=[xt], w=[junk, ss2])
            rstd_from_ss(rstd2, ss2, D, 1)
            dve(lambda e: e.scalar_tensor_tensor(out=h2g.t[:], in0=xt.t[:, j, :], scalar=rstd2.t[:, 0:1], in1=gffn_rep.t[:],
                                                 op0=ALU.mult, op1=ALU.mult), r=[xt, rstd2, gffn_rep], w=[h2g])
            act(lambda e: e.activation(out=h2b.t[:], in_=h2g.t[:], func=AF.Copy), r=[h2g], w=[h2b])
            for kc in range(KD):
                pe(lambda e, kc=kc: e.transpose(psO.t[:, kc * P:(kc + 1) * P], h2g.t[:, kc * P:(kc + 1) * P], ident_f.t[:]),
                   r=[h2g, ident_f], w=[psO])
            dve(lambda e: e.tensor_copy(h2T.t[:], psO.t[:].rearrange("p (k t) -> p k t", k=KD)), r=[psO], w=[h2T])
            for kc in range(KD):
                pe(lambda e, kc=kc: e.matmul(PSR.t[:, 0:36], h2T.t[:, kc, :], wr.t[:, kc, :], start=(kc == 0), stop=False),
                   r=[h2T, wr], w=[PSR])
            pe(lambda e: e.matmul(PSR.t[:, 0:36], ones_row_f.t[0:1, :], br_row.t[0:1, :], start=False, stop=True),
               r=[ones_row_f, br_row], w=[PSR])
            dve(lambda e: e.tensor_copy(lg.t[:], PSR.t[:, 0:36]), r=[PSR], w=[lg])
            R = rt.t
            dve(lambda e: e.tensor_reduce(out=R[:, 0:1], in_=lg.t[:, 0:4], axis=AX.X, op=ALU.max), r=[lg], w=[rt])
            dve(lambda e: e.tensor_scalar(R[:, 1:5], lg.t[:, 0:4], R[:, 0:1], None, op0=ALU.is_ge), r=[lg, rt], w=[rt])
            dve(lambda e: e.tensor_scalar(R[:, 5:9], lg.t[:, 0:4], R[:, 0:1], None, op0=ALU.subtract), r=[lg, rt], w=[rt])
            pool(lambda e: e.tensor_tensor(R[:, 5:9], econst.t[:, 0:4], R[:, 5:9], op=ALU.pow), r=[rt, econst], w=[rt])
            dve(lambda e: e.tensor_reduce(out=R[:, 9:10], in_=R[:, 5:9], axis=AX.X, op=ALU.add), r=[rt], w=[rt])
            dve(lambda e: e.reciprocal(R[:, 10:11], R[:, 9:10]), r=[rt], w=[rt])
            dve(lambda e: e.tensor_scalar(R[:, 11:19], lg.t[:, 4:12], R[:, 1:2], None, op0=ALU.mult), r=[lg, rt], w=[rt])
            for g in range(1, 4):
                dve(lambda e, g=g: e.scalar_tensor_tensor(out=R[:, 11:19], in0=lg.t[:, 4 + 8 * g:12 + 8 * g], scalar=R[:, 1 + g:2 + g],
                                                          in1=R[:, 11:19], op0=ALU.mult, op1=ALU.add), r=[lg, rt], w=[rt])
            dve(lambda e: e.max(R[:, 19:27], R[:, 11:19]), r=[rt], w=[rt])
            dve(lambda e: e.tensor_scalar(R[:, 27:35], R[:, 11:19], R[:, 19:20], None, op0=ALU.is_ge), r=[rt], w=[rt])
            dve(lambda e: e.tensor_scalar(R[:, 35:43], R[:, 11:19], R[:, 20:21], None, op0=ALU.is_ge), r=[rt], w=[rt])
            dve(lambda e: e.tensor_tensor(R[:, 43:44], R[:, 20:21], R[:, 19:20], op=ALU.subtract), r=[rt], w=[rt])
            pool(lambda e: e.tensor_tensor(R[:, 44:45], econst.t[:, 0:1], R[:, 43:44], op=ALU.pow), r=[rt, econst], w=[rt])
            dve(lambda e: e.tensor_scalar(R[:, 45:46], R[:, 44:45], 1.0, None, op0=ALU.add), r=[rt], w=[rt])
            dve(lambda e: e.reciprocal(R[:, 45:46], R[:, 45:46]), r=[rt], w=[rt])
            dve(lambda e: e.tensor_tensor(R[:, 46:47], R[:, 44:45], R[:, 45:46], op=ALU.mult), r=[rt], w=[rt])
            dve(lambda e: e.tensor_scalar(R[:, 47:49], R[:, 45:47], R[:, 10:11], None, op0=ALU.mult), r=[rt], w=[rt])
            M12, M1, M2, pos = W_m = (rt.t[:, 64:96], rt.t[:, 96:128], rt.t[:, 128:160], rt.t[:, 160:192])
            for g in range(4):
                dve(lambda e, g=g: e.tensor_scalar(M12[:, 8 * g:8 * g + 8], R[:, 35:43], R[:, 1 + g:2 + g], None, op0=ALU.mult), r=[rt], w=[rt])
                dve(lambda e, g=g: e.tensor_scalar(M1[:, 8 * g:8 * g + 8], R[:, 27:35], R[:, 1 + g:2 + g], None, op0=ALU.mult), r=[rt], w=[rt])
            dve(lambda e: e.tensor_tensor(M2, M12, M1, op=ALU.subtract), r=[rt], w=[rt])
            pe(lambda e: e.matmul(PSR.t[:, 64:96], tri_f.t[:], M12, start=True, stop=False), r=[tri_f, rt], w=[PSR])
            pe(lambda e: e.matmul(PSR.t[:, 64:96], ones_row_f.t[0:1, :], base_row.t[0:1, :], start=False, stop=True),
               r=[ones_row_f, base_row], w=[PSR])
            pe(lambda e: e.matmul(PSR.t[0:1, 128:160], ones_col_f.t[:, 0:1], M12, start=True, stop=True), r=[ones_col_f, rt], w=[PSR])
            dve(lambda e: e.scalar_tensor_tensor(out=pos, in0=PSR.t[:, 64:96], scalar=-1.0, in1=ebase.t[:], op0=ALU.add, op1=ALU.add),
                r=[PSR, ebase], w=[rt])
            dve(lambda e: e.tensor_scalar(rt.t[:, 192:224], PSR.t[:, 64:96], float(CAP), float(NSLOT), op0=ALU.is_gt, op1=ALU.mult),
                r=[PSR], w=[rt])
            dve(lambda e: e.tensor_tensor(pos, pos, rt.t[:, 192:224], op=ALU.add), r=[rt], w=[rt])
            dve(lambda e: e.tensor_tensor(base_row.t[0:1, :], base_row.t[0:1, :], PSR.t[0:1, 128:160], op=ALU.add), r=[PSR, base_row], w=[base_row])
            dve(lambda e: e.tensor_tensor(rt.t[:, 224:256], M1, pos, op=ALU.mult), r=[rt], w=[rt])
            dve(lambda e: e.tensor_reduce(out=R[:, 49:50], in_=rt.t[:, 224:256], axis=AX.X, op=ALU.add), r=[rt], w=[rt])
            dve(lambda e: e.tensor_tensor(rt.t[:, 224:256], M2, pos, op=ALU.mult), r=[rt], w=[rt])
            dve(lambda e: e.tensor_reduce(out=R[:, 50:51], in_=rt.t[:, 224:256], axis=AX.X, op=ALU.add), r=[rt], w=[rt])
            dve(lambda e: e.tensor_scalar(R[:, 51:53], R[:, 49:51], float(NSLOT) - 0.5, None, op0=ALU.is_lt), r=[rt], w=[rt])
            dve(lambda e: e.tensor_tensor(tokinfo.t[:, tile, 2:4], R[:, 47:49], R[:, 51:53], op=ALU.mult), r=[rt], w=[tokinfo])
            dve(lambda e: e.tensor_scalar(tokinfo.t[:, tile, 0:2], R[:, 49:51], float(NSLOT - 1), None, op0=ALU.min), r=[rt], w=[tokinfo])
            dve(lambda e: e.tensor_copy(tokidx.t[:, tile, :], tokinfo.t[:, tile, 0:2]), r=[tokinfo], w=[tokidx])
            dump("lg", lg.t[:], 36, [lg])
            dump("rt", rt.t[:], 256, [rt])
            dump("h2T0", h2T.t[:, 0, :], P, [h2T])
            sidx = h2b_l[2 + tile % 2]
            dve(lambda e: e.tensor_copy(sidx.t[:], R[:, 49:51]), r=[rt], w=[sidx])
            for k in range(2):
                S.dma("pool", scat_sems[tile % 2],
                      lambda e, k=k: e.indirect_dma_start(out=xs_d[:, :], out_offset=bass.IndirectOffsetOnAxis(ap=sidx.t[:, k:k + 1], axis=0),
                                                          in_=h2b.t[:, :], in_offset=None, bounds_check=bound_reg, oob_is_err=False),
                      r=[h2b, sidx], w=[], serial=True)

        def router_weights(sc, lyr, sem):
            gffn_rep = sb(f"gffn_rep{lyr}", [P, D], F32, sc)
            wr = sb(f"wr{lyr}", [P, KD, 36], F32, sc)
            br_row = sb(f"br_row{lyr}", [1, 36], F32, sc)
            grp = []
            S.dma("sp", sem, lambda e: e.dma_start(out=gffn_rep.t[:], in_=norm_ffn_g[lyr].partition_broadcast(P)), w=[gffn_rep], defer=grp)
            S.dma("sp", sem, lambda e: e.dma_start(out=wr.t[:, :, 0:4], in_=moe_w_rg[lyr].rearrange("(k p) g -> p k g", p=P)), w=[wr], defer=grp)
            S.dma("sp", sem, lambda e: e.dma_start(out=wr.t[:, :, 4:36], in_=moe_w_re[lyr].rearrange("(k p) g -> p k g", p=P)), w=[wr], defer=grp)
            S.dma("sp", sem, lambda e: e.dma_start(out=br_row.t[0:1, 0:4], in_=moe_b_rg[lyr:lyr + 1, :]), w=[br_row], defer=grp)
            S.dma("sp", sem, lambda e: e.dma_start(out=br_row.t[0:1, 4:36], in_=moe_b_re[lyr:lyr + 1, :]), w=[br_row], defer=grp)
            S.flush(grp)
            return gffn_rep, wr, br_row

        def route_bufs(sc, lyr, sem, NJ):
            gffn_rep, wr, br_row = router_weights(sc, lyr, sem)
            ss2 = sb("ss2", [P, 4], F32, sc)
            rstd2 = sb("rstd2", [P, 4], F32, sc)
            junk = sb("junk", [P, D], BF16, sc)
            h2g_l = [sb(f"h2g{i}", [P, D], F32, sc) for i in range(2 if NJ == 4 else 1)]
            h2b_l = [sb(f"h2b{i}", [P, D], BF16, sc) for i in range(NJ)]
            sidx = sb("sidx", [P, 4, 2], I32, sc)
            h2T = sb("h2T", [P, KD, P], F32, sc)
            rt = sb("rt", [P, 1024], F32, sc)
            scat_sems = [S.newsem() for _ in range(NJ)]
            pool(lambda e: e.memset(base_row.t[:], 0.0), w=[base_row])
            return (gffn_rep, wr, br_row, ss2, rstd2, junk, h2g_l, h2b_l, h2T, ps[2], sidx, rt, scat_sems)

        pending_scatter = []

        def flush_scatters():
            while pending_scatter:
                pending_scatter.pop(0)()

        def route_batch(lyr, xt, tile0, NJ, W):
            (gffn_rep, wr, br_row, ss2, rstd2, junk, h2g_l, h2b_l, h2T, PSR, sidx, rt, scat_sems) = W
            R = rt.t
            for j in range(NJ):
                act(lambda e, j=j: e.activation(out=junk.t[:], in_=xt.t[:, j, :], func=AF.Square, accum_out=ss2.t[:, j:j + 1]), r=[xt], w=[junk, ss2])
            rstd_from_ss(rstd2, ss2, D, NJ)
            flush_scatters()
            for j in range(NJ):
                h2g = h2g_l[j % len(h2g_l)]
                h2b = h2b_l[j]
                dve(lambda e, j=j, h2g=h2g: e.scalar_tensor_tensor(out=h2g.t[:], in0=xt.t[:, j, :], scalar=rstd2.t[:, j:j + 1], in1=gffn_rep.t[:],
                                                                   op0=ALU.mult, op1=ALU.mult), r=[xt, rstd2, gffn_rep], w=[h2g])
                act(lambda e, h2g=h2g, h2b=h2b: e.activation(out=h2b.t[:], in_=h2g.t[:], func=AF.Copy), r=[h2g], w=[h2b])
                for kc in range(KD):
                    pe(lambda e, kc=kc, h2g=h2g: e.transpose(psO.t[:, kc * P:(kc + 1) * P], h2g.t[:, kc * P:(kc + 1) * P], ident_f.t[:]),
                       r=[h2g, ident_f], w=[psO])
                if j % 2 == 0:
                    dve(lambda e: e.tensor_copy(h2T.t[:], psO.t[:].rearrange("p (k t) -> p k t", k=KD)), r=[psO], w=[h2T])
                else:
                    act(lambda e: e.activation(out=h2T.t[:], in_=psO.t[:].rearrange("p (k t) -> p k t", k=KD), func=AF.Copy), r=[psO], w=[h2T])
                for kc in range(KD):
                    pe(lambda e, kc=kc, j=j: e.matmul(PSR.t[:, j * 36:(j + 1) * 36], h2T.t[:, kc, :], wr.t[:, kc, :], start=(kc == 0), stop=False),
                       r=[h2T, wr], w=[PSR])
                pe(lambda e, j=j: e.matmul(PSR.t[:, j * 36:(j + 1) * 36], ones_row_f.t[0:1, :], br_row.t[0:1, :], start=False, stop=True),
                   r=[ones_row_f, br_row], w=[PSR])
                yield
            J4, J8, J32 = NJ * 4, NJ * 8, NJ * 32
            lg3 = R[:, 0:NJ * 36].rearrange("p (j c) -> p j c", j=NJ)
            gmax = R[:, 144:144 + NJ]
            goh = R[:, 148:148 + J4].rearrange("p (j g) -> p j g", j=NJ)
            d4f = R[:, 164:164 + J4]
            d4 = d4f.rearrange("p (j g) -> p j g", j=NJ)
            gsum = R[:, 180:180 + NJ]
            gprob = R[:, 184:184 + NJ]
            prodf = R[:, 192:192 + J32]
            prod = prodf.rearrange("p (j g e) -> p j g e", j=NJ, g=4)
            esel = R[:, 320:320 + J8].rearrange("p (j e) -> p j e", j=NJ)
            top8 = R[:, 352:352 + J8].rearrange("p (j e) -> p j e", j=NJ)
            s1 = R[:, 384:384 + J8].rearrange("p (j e) -> p j e", j=NJ)
            s12 = R[:, 416:416 + J8].rearrange("p (j e) -> p j e", j=NJ)
            dm = R[:, 448:448 + NJ]
            ex = R[:, 452:452 + NJ]
            p1 = R[:, 456:456 + NJ]
            p2 = R[:, 460:460 + NJ]
            gates = R[:, 464:464 + 2 * NJ].rearrange("p (j k) -> p j k", j=NJ)
            M12f, M1f, M2f, posf = R[:, 480:480 + J32], R[:, 608:608 + J32], R[:, 736:736 + J32], R[:, 864:864 + J32]
            dest = R[:, 992:992 + 2 * NJ].rearrange("p (j k) -> p j k", j=NJ)
            valid = R[:, 1000:1000 + 2 * NJ].rearrange("p (j k) -> p j k", j=NJ)
            gmax_b = gmax.unsqueeze(2).broadcast_to([P, NJ, 4])

            def rdve(fn, extra_r=(), extra_w=()):
                dve(fn, r=[rt] + list(extra_r), w=[rt] + list(extra_w))

            rdve(lambda e: e.tensor_copy(R[:, 0:NJ * 36], PSR.t[:, 0:NJ * 36]), extra_r=[PSR])
            rdve(lambda e: e.tensor_reduce(out=gmax, in_=lg3[:, :, 0:4], axis=AX.X, op=ALU.max))
            rdve(lambda e: e.tensor_tensor(goh, lg3[:, :, 0:4], gmax_b, op=ALU.is_ge))
            rdve(lambda e: e.tensor_tensor(d4, lg3[:, :, 0:4], gmax_b, op=ALU.subtract))
            pool(lambda e: e.tensor_tensor(d4f, econst.t[:, 0:J4], d4f, op=ALU.pow), r=[rt, econst], w=[rt])
            yield
            rdve(lambda e: e.tensor_reduce(out=gsum, in_=d4, axis=AX.X, op=ALU.add))
            rdve(lambda e: e.reciprocal(gprob, gsum))
            rdve(lambda e: e.tensor_tensor(prod, lg3[:, :, 4:36].rearrange("p j (g e) -> p j g e", g=4), goh.unsqueeze(3).broadcast_to([P, NJ, 4, 8]), op=ALU.mult))
            rdve(lambda e: e.tensor_reduce(out=esel, in_=prod.rearrange("p j g e -> p j e g"), axis=AX.X, op=ALU.add))
            for j in range(NJ):
                rdve(lambda e, j=j: e.max(top8[:, j, :], esel[:, j, :]))
            yield
            rdve(lambda e: e.tensor_tensor(s1, esel, top8[:, :, 0:1].broadcast_to([P, NJ, 8]), op=ALU.is_ge))
            rdve(lambda e: e.tensor_tensor(s12, esel, top8[:, :, 1:2].broadcast_to([P, NJ, 8]), op=ALU.is_ge))
            rdve(lambda e: e.tensor_tensor(dm, top8[:, :, 1], top8[:, :, 0], op=ALU.subtract))
            pool(lambda e: e.tensor_tensor(ex, econst.t[:, 0:NJ], dm, op=ALU.pow), r=[rt, econst], w=[rt])
            yield
            rdve(lambda e: e.tensor_scalar(p1, ex, 1.0, None, op0=ALU.add))
            rdve(lambda e: e.reciprocal(p1, p1))
            rdve(lambda e: e.tensor_tensor(p2, ex, p1, op=ALU.mult))
            rdve(lambda e: e.tensor_tensor(gates[:, :, 0], gprob, p1, op=ALU.mult))
            rdve(lambda e: e.tensor_tensor(gates[:, :, 1], gprob, p2, op=ALU.mult))
            goh_b = goh.unsqueeze(3).broadcast_to([P, NJ, 4, 8])
            rdve(lambda e: e.tensor_tensor(M12f.rearrange("p (j g e) -> p j g e", j=NJ, g=4), goh_b, s12.unsqueeze(2).broadcast_to([P, NJ, 4, 8]), op=ALU.mult))
            rdve(lambda e: e.tensor_tensor(M1f.rearrange("p (j g e) -> p j g e", j=NJ, g=4), goh_b, s1.unsqueeze(2).broadcast_to([P, NJ, 4, 8]), op=ALU.mult))
            rdve(lambda e: e.tensor_tensor(M2f, M12f, M1f, op=ALU.subtract))
            yield
            C0 = 192
            for j in range(NJ):
                oj = PSR.t[:, C0 + j * NE:C0 + (j + 1) * NE]
                pe(lambda e, j=j, oj=oj: e.matmul(oj, tri_f.t[:], M12f[:, j * NE:(j + 1) * NE], start=True, stop=False), r=[tri_f, rt], w=[PSR])
                for j2 in range(j):
                    pe(lambda e, j2=j2, oj=oj: e.matmul(oj, ones_mat.t[:], M12f[:, j2 * NE:(j2 + 1) * NE], start=False, stop=False), r=[ones_mat, rt], w=[PSR])
                pe(lambda e, oj=oj: e.matmul(oj, ones_row_f.t[0:1, :], base_row.t[0:1, :], start=False, stop=True), r=[ones_row_f, base_row], w=[PSR])
            pe(lambda e: e.matmul(PSR.t[0:1, 384:384 + J32], ones_col_f.t[:, 0:1], M12f, start=True, stop=True), r=[ones_col_f, rt], w=[PSR])
            cum = PSR.t[:, C0:C0 + J32]
            yield
            rdve(lambda e: e.scalar_tensor_tensor(out=posf, in0=cum, scalar=-1.0, in1=ebaseN.t[:, 0:J32], op0=ALU.add, op1=ALU.add), extra_r=[PSR, ebaseN])
            rdve(lambda e: e.tensor_scalar(prodf, cum, float(CAP), float(NSLOT), op0=ALU.is_gt, op1=ALU.mult), extra_r=[PSR])
            rdve(lambda e: e.tensor_tensor(posf, posf, prodf, op=ALU.add))
            rdve(lambda e: e.tensor_reduce(out=R[0:1, 320:320 + NE], in_=PSR.t[0:1, 384:384 + J32].rearrange("p (j e) -> p e j", j=NJ), axis=AX.X, op=ALU.add), extra_r=[PSR])
            dve(lambda e: e.tensor_tensor(base_row.t[0:1, :], base_row.t[0:1, :], R[0:1, 320:320 + NE], op=ALU.add), r=[rt, base_row], w=[base_row])
            rdve(lambda e: e.tensor_tensor(prodf, M1f, posf, op=ALU.mult))
            rdve(lambda e: e.tensor_reduce(out=dest[:, :, 0], in_=prodf.rearrange("p (j e) -> p j e", j=NJ), axis=AX.X, op=ALU.add))
            rdve(lambda e: e.tensor_tensor(prodf, M2f, posf, op=ALU.mult))
            rdve(lambda e: e.tensor_reduce(out=dest[:, :, 1], in_=prodf.rearrange("p (j e) -> p j e", j=NJ), axis=AX.X, op=ALU.add))
            rdve(lambda e: e.tensor_scalar(valid, dest, float(NSLOT) - 0.5, None, op0=ALU.is_lt))
            dve(lambda e: e.tensor_tensor(tokinfo.t[:, tile0:tile0 + NJ, 2:4], gates, valid, op=ALU.mult), r=[rt], w=[tokinfo])
            dve(lambda e: e.tensor_scalar(tokinfo.t[:, tile0:tile0 + NJ, 0:2], dest, float(NSLOT - 1), None, op0=ALU.min), r=[rt], w=[tokinfo])
            dve(lambda e: e.tensor_copy(tokidx.t[:, tile0:tile0 + NJ, :], tokinfo.t[:, tile0:tile0 + NJ, 0:2]), r=[tokinfo], w=[tokidx])
            dve(lambda e: e.tensor_copy(sidx.t[:, 0:NJ, :], dest), r=[rt], w=[sidx])
            yield
            def emit_scatters():
                for j in range(NJ):
                    for k in range(2):
                        S.dma("pool", scat_sems[j],
                              lambda e, j=j, k=k: e.indirect_dma_start(out=xs_d[:, :], out_offset=bass.IndirectOffsetOnAxis(ap=sidx.t[:, j, k:k + 1], axis=0),
                                                                       in_=h2b_l[j].t[:, :], in_offset=None, bounds_check=bound_reg, oob_is_err=False),
                              r=[h2b_l[j], sidx, xs_zero], w=[], serial=True)
            pending_scatter.append(emit_scatters)

        def pass1():
            with ExitStack() as sc:
                curscope[0] = sc
                wsem = S.newsem()
                stgsems = [S.newsem(), S.newsem()]
                w_in = sb("w_in", [P, KD, 2560], BF16, sc)
                w_out = sb("w_out", [P, KD, D], BF16, sc)
                gmix = sb("gmix", [P, KD], F32, sc)
                wsT = sb("wsT", [P, 4, P], BF16, sc)
                wsb_b = sb("wsb_b", [1, 512], BF16, sc)
                alng = sb("alng", [P, 512], F32, sc)
                bconv = sb("bconv", [P, 4, 3], F32, sc)
                tmpsc = ExitStack()
                stg = [sb(f"stg{i}", [P, 2560], F32, tmpsc) for i in range(2)]
                ws_raw = sb("ws_raw", [P, 4, P], F32, tmpsc)
                wsb_f = sb("wsb_f", [1, 512], F32, tmpsc)
                grp = []
                S.dma("sp", wsem, lambda e: e.dma_start(out=gmix.t[:], in_=norm_mix_g[0].rearrange("(k p) -> p k", p=P), allow_slow_non_contiguous=True), w=[gmix], defer=grp)
                S.dma("sp", wsem, lambda e: e.dma_start(out=ws_raw.t[:], in_=a_ws[0].rearrange("h t s -> t h s")), w=[ws_raw], defer=grp)
                S.dma("sp", wsem, lambda e: e.dma_start(out=wsb_f.t[0:1, :], in_=a_ws_b[0].rearrange("(o h) t -> o (h t)", o=1)), w=[wsb_f], defer=grp)
                S.dma("sp", wsem, lambda e: e.dma_start(out=alng.t[:], in_=a_ln_g[0].partition_broadcast(P)), w=[alng], defer=grp)
                for k_ in range(3):
                    S.dma("sp", wsem, lambda e, k_=k_: e.dma_start(out=bconv.t[:, :, k_], in_=b_conv_w[0, k_].rearrange("(c p) -> p c", p=P), allow_slow_non_contiguous=True), w=[bconv], defer=grp)
                S.flush(grp)
                load_cast_rows(stg, stgsems, w_in, lambda kc: ab_w_in[0, kc * P:(kc + 1) * P, :], KD, 2560, gmix)
                load_cast_rows(stg, stgsems, w_out, lambda kc: ab_w_out[0, kc * P:(kc + 1) * P, :], KD, D, None)
                dve(lambda e: e.tensor_copy(wsb_b.t[:], wsb_f.t[:]), r=[wsb_f], w=[wsb_b])
                for h in range(4):
                    pe(lambda e, h=h: e.transpose(ps[5].t[:, h * P:(h + 1) * P], ws_raw.t[:, h, :], ident_f.t[:]), r=[ws_raw, ident_f], w=[ps[5]])
                    dve(lambda e, h=h: e.tensor_tensor(wsT.t[:, h, :], ps[5].t[:, h * P:(h + 1) * P], tri_f.t[:], op=ALU.mult), r=[ps[5], tri_f], w=[wsT])
                S.barrier()
                tmpsc.close()
                ztile = sb("ztile", [P, D], BF16, sc)
                zsem = S.newsem()
                dve(lambda e: e.memset(ztile.t[:], 0.0), w=[ztile])
                zgrp = []
                ZR = 8
                for r_ in range(0, NSLOT, P * ZR):
                    S.dma("act", zsem, lambda e, r_=r_: e.dma_start(out=xs_d[r_:r_ + P * ZR, :].rearrange("(s p) d -> p s d", p=P),
                                                                   in_=ztile.t[:].unsqueeze(1).broadcast_to([P, ZR, D])), r=[ztile], w=[xs_zero], defer=zgrp)
                S.flush(zgrp)
                RW = route_bufs(sc, 0, wsem, MT1 // P)
                junk = RW[5]
                NJ = MT1 // P
                xt_l = [sb(f"xt{i}", [P, NJ, D], F32, sc) for i in range(2)]
                xsem = [S.newsem(), S.newsem()]
                stsem = [S.newsem(), S.newsem()]
                hb_l = [sb(f"hb{i}", [P, D], BF16, sc) for i in range(2)]
                hT = sb("hT", [P, KD, MT1], BF16, sc)
                ss = sb("ss", [P, NJ], F32, sc)
                rstd = sb("rstd", [P, NJ], F32, sc)
                ug = sb("ug", [P, 4, MT1], BF16, sc)
                vg_l = [sb(f"vg{i}", [P, 512], F32, sc) for i in range(2)]
                vn_l = [sb(f"vn{i}", [P, 512], BF16, sc) for i in range(2)]
                st6 = sb("st6", [P, 6], F32, sc)
                mv = sb("mv", [P, 4], F32, sc)
                xb_sb = sb("xb_sb", [P, MT1], F32, sc)
                cx = sb("cx", [P, 4, MT1 + 2], F32, sc)
                cv_l = [sb(f"cv{i}", [P, MT1], F32, sc) for i in range(2)]
                yT_l = [sb(f"yT{i}", [P, KD, MT1], BF16, sc) for i in range(2)]
                pool(lambda e: e.memset(cx.t[:], 0.0), w=[cx])
                zrot = [0]

                def zchunk(col0):
                    pz = ps[zrot[0] % 2]
                    zrot[0] += 1
                    for kc in range(KD):
                        pe(lambda e, kc=kc: e.matmul(pz.t[:, 0:MT1], w_in.t[:, kc, col0:col0 + P], hT.t[:, kc, :], start=(kc == 0), stop=(kc == KD - 1)),
                           r=[w_in, hT], w=[pz])
                    return pz

                NM = N // MT1

                def load1(m):
                    xt = xt_l[m % 2]
                    S.dma("sp", xsem[m % 2], lambda e, m=m, xt=xt: e.dma_start(out=xt.t[:], in_=x_d[m * MT1:(m + 1) * MT1, :].rearrange("(j p) d -> p j d", p=P)), w=[xt])

                def front1(m):
                    xt = xt_l[m % 2]
                    yT = yT_l[m % 2]
                    norm_and_transpose(sc, xt, NJ, hb_l, hT, ss, rstd, junk, ps[3])
                    yield
                    for c in range(4):
                        pz = zchunk(c * P)
                        act(lambda e, c=c, pz=pz: e.activation(out=ug.t[:, c, :], in_=pz.t[:, 0:MT1], func=AF.Gelu_apprx_tanh), r=[pz], w=[ug])
                        yield
                        pass
                    def vmm(j):
                        pv = ps[4] if j % 2 == 0 else ps[3]
                        for kc in range(KD):
                            pe(lambda e, kc=kc, j=j, pv=pv: e.matmul(pv.t[:, 0:512], hT.t[:, kc, j * P:(j + 1) * P], w_in.t[:, kc, 512:1024], start=(kc == 0), stop=(kc == KD - 1)),
                               r=[hT, w_in], w=[pv])

                    vmm(0)
                    for j in range(NJ):
                        vg = vg_l[j % 2]
                        vn = vn_l[j % 2]
                        pv = ps[4] if j % 2 == 0 else ps[3]
                        if j + 1 < NJ:
                            vmm(j + 1)
                        act(lambda e, vg=vg, pv=pv: e.activation(out=vg.t[:], in_=pv.t[:, 0:512], func=AF.Gelu_apprx_tanh, accum_out=st6.t[:, 0:1]), r=[pv], w=[vg, st6])
                        dump("vg", vg.t[:], 512, [vg])
                        act(lambda e, vg=vg: e.activation(out=junk.t[:, 0:512], in_=vg.t[:], func=AF.Square, accum_out=st6.t[:, 1:2]), r=[vg], w=[junk, st6])
                        pool(lambda e: e.tensor_scalar(mv.t[:, 0:1], st6.t[:, 0:1], 1.0 / 512, None, op0=ALU.mult), r=[st6], w=[mv])
                        pool(lambda e: e.tensor_tensor(mv.t[:, 1:2], mv.t[:, 0:1], mv.t[:, 0:1], op=ALU.mult), r=[mv], w=[mv])
                        pool(lambda e: e.tensor_scalar(mv.t[:, 2:3], st6.t[:, 1:2], 1.0 / 512, EPS, op0=ALU.mult, op1=ALU.add), r=[st6, mv], w=[mv])
                        pool(lambda e: e.tensor_tensor(mv.t[:, 2:3], mv.t[:, 2:3], mv.t[:, 1:2], op=ALU.subtract), r=[mv], w=[mv])
                        pool(lambda e: e.tensor_tensor(mv.t[:, 2:3], mv.t[:, 2:3], mhalf.t[:, 0:1], op=ALU.pow), r=[mv, mhalf], w=[mv])
                        dve(lambda e, vg=vg: e.tensor_scalar(vg.t[:], vg.t[:], mv.t[:, 0:1], mv.t[:, 2:3], op0=ALU.subtract, op1=ALU.mult), r=[vg, mv], w=[vg])
                        pool(lambda e, vg=vg, vn=vn: e.tensor_tensor(vn.t[:], vg.t[:], alng.t[:], op=ALU.mult), r=[vg, alng], w=[vn])
                        dump("mv", mv.t[:], 4, [mv])
                        dump("vn", vn.t[:], 512, [vn])
                        for h in range(4):
                            pe(lambda e, h=h, vn=vn: e.matmul(ps[5].t[:, h * P:(h + 1) * P], vn.t[:, h * P:(h + 1) * P], wsT.t[:, h, :], start=True, stop=False),
                               r=[vn, wsT], w=[ps[5]])
                            pe(lambda e, h=h: e.matmul(ps[5].t[:, h * P:(h + 1) * P], ones_row_b.t[0:1, :], wsb_b.t[0:1, h * P:(h + 1) * P], start=False, stop=True),
                               r=[ones_row_b, wsb_b], w=[ps[5]])
                        dve(lambda e, j=j: e.tensor_tensor(yT.t[:, 0:4, j * P:(j + 1) * P], ug.t[:, :, j * P:(j + 1) * P],
                                                           ps[5].t[:].rearrange("p (h t) -> p h t", h=4), op=ALU.mult), r=[ug, ps[5]], w=[yT])
                        yield
                        dump("pm", ps[5].t[:], 512, [ps[5]])
                        dump("yTa", yT.t[:, 0, 0:P], P, [yT])
                    for c in range(4):
                        cv = cv_l[c % 2]
                        pxb = zchunk(2048 + c * P)
                        act(lambda e, pxb=pxb: e.activation(out=xb_sb.t[:], in_=pxb.t[:, 0:MT1], func=AF.Copy), r=[pxb], w=[xb_sb])
                        pgc = zchunk(1536 + c * P)
                        dve(lambda e, c=c, pgc=pgc: e.tensor_tensor(cx.t[:, c, 2:MT1 + 2], pgc.t[:, 0:MT1], xb_sb.t[:], op=ALU.mult), r=[pgc, xb_sb], w=[cx])
                        act(lambda e, c=c, cv=cv: e.activation(out=cv.t[:], in_=cx.t[:, c, 2:MT1 + 2], func=AF.Identity, scale=bconv.t[:, c, 2:3]), r=[cx, bconv], w=[cv])
                        dve(lambda e, c=c, cv=cv: e.scalar_tensor_tensor(out=cv.t[:], in0=cx.t[:, c, 1:MT1 + 1], scalar=bconv.t[:, c, 1:2], in1=cv.t[:], op0=ALU.mult, op1=ALU.add),
                             r=[cx, bconv, cv], w=[cv])
                        dve(lambda e, c=c, cv=cv: e.scalar_tensor_tensor(out=cv.t[:], in0=cx.t[:, c, 0:MT1], scalar=bconv.t[:, c, 0:1], in1=cv.t[:], op0=ALU.mult, op1=ALU.add),
                             r=[cx, bconv, cv], w=[cv])
                        pool(lambda e, c=c: e.tensor_copy(cx.t[:, c, 0:2], cx.t[:, c, MT1:MT1 + 2]), r=[cx], w=[cx])
                        pgb = zchunk(1024 + c * P)
                        dve(lambda e, c=c, pgb=pgb, cv=cv: e.tensor_tensor(yT.t[:, 4 + c, :], pgb.t[:, 0:MT1], cv.t[:], op=ALU.mult), r=[pgb, cv], w=[yT])
                        yield
                        pass
                        pass
                def back1(m):
                    xt = xt_l[m % 2]
                    yT = yT_l[m % 2]
                    for j in range(NJ):
                        tile = m * NJ + j
                        for half in range(2):
                            for c in range(KD):
                                pe(lambda e, c=c, half=half, j=j: e.matmul(psO.t[:, half * 512:(half + 1) * 512], yT.t[:, c, j * P:(j + 1) * P],
                                                                          w_out.t[:, c, half * 512:(half + 1) * 512], start=(c == 0), stop=(c == KD - 1)),
                                   r=[yT, w_out], w=[psO])
                        dve(lambda e, j=j, xt=xt: e.tensor_tensor(xt.t[:, j, :], xt.t[:, j, :], psO.t[:], op=ALU.add), r=[xt, psO], w=[xt])
                        yield
                    yield from route_batch(0, xt, m * NJ, NJ, RW)
                    S.dma("sp", stsem[m % 2], lambda e, m=m, xt=xt: e.dma_start(out=xres_d[m * MT1:(m + 1) * MT1, :].rearrange("(j p) d -> p j d", p=P), in_=xt.t[:]),
                          r=[xt], w=[])

                load1(0)
                if NM > 1:
                    load1(1)
                interleave(front1(0))
                for m in range(NM):
                    interleave(front1(m + 1) if m + 1 < NM else None, back1(m))
                    if m + 2 < NM:
                        load1(m + 2)
                flush_scatters()
                S.barrier()

        def pass_experts(lyr):
            with ExitStack() as sc:
                curscope[0] = sc
                stg = [sb(f"estg{i}", [P, 4096], F32, sc) for i in range(3)]
                stgsem = [S.newsem() for _ in range(3)]
                wg_l = [sb(f"wg{i}", [P, KD, 512], BF16, sc) for i in range(2)]
                wu_l = [sb(f"wu{i}", [P, KD, 512], BF16, sc) for i in range(2)]
                wd_l = [sb(f"wd{i}", [P, 4, D], BF16, sc) for i in range(2)]
                xe_l = [sb(f"xe{i}", [P, 4, D], BF16, sc) for i in range(2)]
                xesem = [S.newsem(), S.newsem()]
                XT = sb("XT", [P, KD, 512], BF16, sc)
                sg_l = [sb(f"sg{i}", [P, 512], F32, sc) for i in range(2)]
                hidT = sb("hidT", [P, 4, 512], BF16, sc)
                ye_l = [sb(f"ye{i}", [P, D], BF16, sc) for i in range(2)]
                yesem = [S.newsem(), S.newsem()]
                psO_h = [Buf("psO_h0"), Buf("psO_h1")]
                blocks = []
                r0 = 0
                while r0 < CAP:
                    nb = min(4, (CAP - r0) // P)
                    blocks.append((r0, nb))
                    r0 += nb * P
                items = [(ex, r0, nb) for ex in range(NE) for (r0, nb) in blocks]
                scnt = [0]

                def emit_w(ex):
                    wg, wu, wd = wg_l[ex % 2], wu_l[ex % 2], wd_l[ex % 2]
                    for wi, (dst, src, nk) in enumerate(((wg, moe_w_gate[lyr, ex].rearrange("(k p) f -> p k f", p=P), KD),
                                                         (wu, moe_w_up[lyr, ex].rearrange("(k p) f -> p k f", p=P), KD),
                                                         (wd, moe_w_down[lyr, ex].rearrange("(k p) f -> p k f", p=P), 4))):
                        st = stg[scnt[0] % 3]
                        sem = stgsem[scnt[0] % 3]
                        scnt[0] += 1
                        S.dma("sp", sem, lambda e, st=st, src=src, nk=nk: e.dma_start(out=st.t[:].rearrange("p (k f) -> p k f", k=nk), in_=src), w=[st])
                        if wi == 0:
                            act(lambda e, st=st, dst=dst, nk=nk: e.activation(out=dst.t[:], in_=st.t[:].rearrange("p (k f) -> p k f", k=nk), func=AF.Copy), r=[st], w=[dst])
                        elif wi == 1:
                            pool(lambda e, st=st, dst=dst, nk=nk: e.tensor_copy(dst.t[:], st.t[:].rearrange("p (k f) -> p k f", k=nk)), r=[st], w=[dst])
                        else:
                            dve(lambda e, st=st, dst=dst, nk=nk: e.tensor_copy(dst.t[:], st.t[:].rearrange("p (k f) -> p k f", k=nk)), r=[st], w=[dst])

                def emit_x(i):
                    ex, r0, nb = items[i]
                    xe = xe_l[i % 2]
                    row0 = ex * CAP + r0
                    S.dma("sp", xesem[i % 2], lambda e: e.dma_start(out=xe.t[:, 0:nb, :], in_=xs_d[row0:row0 + nb * P, :].rearrange("(s p) d -> p s d", p=P)),
                          r=[xs_b], w=[xe])

                XT_l = [XT, sb("XTb", [P, KD, 512], BF16, sc)]
                ycnt = [0]

                def gen_transposes(i):
                    ex, r0, nb = items[i]
                    xe = xe_l[i % 2]
                    XTi = XT_l[i % 2]
                    for s_ in range(nb):
                        ptb = ps[3] if s_ % 2 == 0 else ps[5]
                        ptv = ptb.t[:].bitcast(BF16)
                        for kc in range(KD):
                            pe(lambda e, kc=kc, s_=s_, xe=xe, ptv=ptv: e.transpose(ptv[:, kc * P:(kc + 1) * P], xe.t[:, s_, kc * P:(kc + 1) * P], ident_b.t[:]),
                               r=[xe, ident_b], w=[ptb])
                        if s_ % 2 == 0:
                            dve(lambda e, s_=s_, ptv=ptv: e.tensor_copy(XTi.t[:, :, s_ * P:(s_ + 1) * P], ptv.rearrange("p (k t) -> p k t", k=KD)), r=[ptb], w=[XTi])
                        else:
                            act(lambda e, s_=s_, ptv=ptv: e.activation(out=XTi.t[:, :, s_ * P:(s_ + 1) * P], in_=ptv.rearrange("p (k t) -> p k t", k=KD), func=AF.Copy), r=[ptb], w=[XTi])
                        yield

                def gen_compute(i):
                    ex, r0, nb = items[i]
                    wg, wu, wd = wg_l[ex % 2], wu_l[ex % 2], wd_l[ex % 2]
                    XTi = XT_l[i % 2]
                    row0 = ex * CAP + r0
                    W_ = nb * P
                    for fc in range(4):
                        pg = ps[0] if fc % 2 == 0 else ps[2]
                        pu = ps[1] if fc % 2 == 0 else ps[4]
                        sg = sg_l[fc % 2]
                        for kc in range(KD):
                            pe(lambda e, kc=kc, fc=fc, pg=pg: e.matmul(pg.t[:, 0:W_], wg.t[:, kc, fc * P:(fc + 1) * P], XTi.t[:, kc, 0:W_], start=(kc == 0), stop=(kc == KD - 1)),
                               r=[wg, XTi], w=[pg])
                        for kc in range(KD):
                            pe(lambda e, kc=kc, fc=fc, pu=pu: e.matmul(pu.t[:, 0:W_], wu.t[:, kc, fc * P:(fc + 1) * P], XTi.t[:, kc, 0:W_], start=(kc == 0), stop=(kc == KD - 1)),
                               r=[wu, XTi], w=[pu])
                        act(lambda e, pg=pg, sg=sg: e.activation(out=sg.t[:, 0:W_], in_=pg.t[:, 0:W_], func=AF.Silu), r=[pg], w=[sg])
                        dve(lambda e, fc=fc, pu=pu, sg=sg: e.tensor_tensor(hidT.t[:, fc, 0:W_], sg.t[:, 0:W_], pu.t[:, 0:W_], op=ALU.mult), r=[sg, pu], w=[hidT])
                        yield
                    for s_ in range(nb):
                        ye = ye_l[ycnt[0] % 2]
                        sem = yesem[ycnt[0] % 2]
                        ycnt[0] += 1
                        for half in range(2):
                            hb_ = psO_h[half]
                            for fc in range(4):
                                pe(lambda e, fc=fc, half=half, s_=s_: e.matmul(psO.t[:, half * 512:(half + 1) * 512], hidT.t[:, fc, s_ * P:(s_ + 1) * P],
                                                                             wd.t[:, fc, half * 512:(half + 1) * 512], start=(fc == 0), stop=(fc == 3)),
                                   r=[hidT, wd], w=[hb_])
                            if half == 0:
                                act(lambda e, ye=ye: e.activation(out=ye.t[:, 0:512], in_=psO.t[:, 0:512], func=AF.Copy), r=[hb_], w=[ye])
                            else:
                                dve(lambda e, ye=ye: e.tensor_copy(ye.t[:, 512:1024], psO.t[:, 512:1024]), r=[hb_], w=[ye])
                        S.dma("act", sem, lambda e, ye=ye, row0=row0, s_=s_: e.dma_start(out=ys_d[row0 + s_ * P:row0 + (s_ + 1) * P, :], in_=ye.t[:]), r=[ye], w=[])
                        yield

                emit_w(0)
                emit_x(0)
                if len(items) > 1:
                    emit_x(1)
                interleave(gen_transposes(0))
                for i, (ex, r0, nb) in enumerate(items):
                    if r0 == 0 and ex + 1 < NE:
                        emit_w(ex + 1)
                    interleave(gen_compute(i), gen_transposes(i + 1) if i + 1 < len(items) else None)
                    if i + 2 < len(items):
                        emit_x(i + 2)
                S.barrier()

        def combine(xt, j, tile, yg_l, ygsem, cnt):
            yg = yg_l[cnt % 2]
            sem = ygsem[cnt % 2]
            for k in range(2):
                S.dma("pool", sem, lambda e, k=k, yg=yg: e.indirect_dma_start(out=yg.t[:, k, :], out_offset=None, in_=ys_d[:, :],
                                                                               in_offset=bass.IndirectOffsetOnAxis(ap=tokidx.t[:, tile, k:k + 1], axis=0)),
                      r=[ys_b, tokidx], w=[yg], serial=True)
            for k in range(2):
                dve(lambda e, k=k, yg=yg: e.scalar_tensor_tensor(out=xt.t[:, j, :], in0=yg.t[:, k, :], scalar=tokinfo.t[:, tile, 2 + k:3 + k], in1=xt.t[:, j, :],
                                                                 op0=ALU.mult, op1=ALU.add), r=[yg, tokinfo, xt], w=[xt])

        def pass3():
            with ExitStack() as sc:
                curscope[0] = sc
                wsem = S.newsem()
                stgsems = [S.newsem(), S.newsem()]
                w_in = sb("cw_in", [P, KD, 2048], BF16, sc)
                w_out = sb("cw_out", [P, KD, D], BF16, sc)
                gmix = sb("gmix1", [P, KD], F32, sc)
                wa = sb("wa", [P, 8, P], BF16, sc)
                wx = sb("wx", [P, 8, P], BF16, sc)
                cw = sb("cw", [P, KD, 4], F32, sc)
                cvec = sb("cvec", [P, 8, KD], F32, sc)
                tmpsc = ExitStack()
                stg = [sb(f"stg3{i}", [P, 2048], F32, tmpsc) for i in range(2)]
                wa_f = sb("wa_f", [P, 8, P], F32, tmpsc)
                wx_f = sb("wx_f", [P, 8, P], F32, tmpsc)
                grp = []
                S.dma("sp", wsem, lambda e: e.dma_start(out=gmix.t[:], in_=norm_mix_g[1].rearrange("(k p) -> p k", p=P), allow_slow_non_contiguous=True), w=[gmix], defer=grp)
                S.dma("sp", wsem, lambda e: e.dma_start(out=wa_f.t[:], in_=c_w_a[0].rearrange("h i j -> i h j")), w=[wa_f], defer=grp)
                S.dma("sp", wsem, lambda e: e.dma_start(out=wx_f.t[:], in_=c_w_x[0].rearrange("h i j -> i h j")), w=[wx_f], defer=grp)
                for k_ in range(4):
                    S.dma("sp", wsem, lambda e, k_=k_: e.dma_start(out=cw.t[:, :, k_], in_=c_conv_w[0, k_].rearrange("(c p) -> p c", p=P), allow_slow_non_contiguous=True), w=[cw], defer=grp)
                for i_, src in enumerate((c_conv_b, c_b_a, c_b_x, c_lambda)):
                    S.dma("sp", wsem, lambda e, i_=i_, src=src: e.dma_start(out=cvec.t[:, i_, :], in_=src[0].rearrange("(c p) -> p c", p=P), allow_slow_non_contiguous=True), w=[cvec], defer=grp)
                S.flush(grp)
                dve(lambda e: e.tensor_copy(wa.t[:], wa_f.t[:]), r=[wa_f], w=[wa])
                dve(lambda e: e.tensor_copy(wx.t[:], wx_f.t[:]), r=[wx_f], w=[wx])
                act(lambda e: e.activation(out=cvec.t[:, 4, :], in_=cvec.t[:, 3, :], func=AF.Exp, scale=-1.0), r=[cvec], w=[cvec])
                dve(lambda e: e.tensor_scalar(cvec.t[:, 6, :], cvec.t[:, 4, :], 1.0 / 3.0, -0.5, op0=ALU.mult, op1=ALU.add), r=[cvec], w=[cvec])
                dve(lambda e: e.tensor_tensor(cvec.t[:, 6, :], cvec.t[:, 6, :], cvec.t[:, 4, :], op=ALU.mult), r=[cvec], w=[cvec])
                dve(lambda e: e.tensor_scalar(cvec.t[:, 6, :], cvec.t[:, 6, :], 1.0, None, op0=ALU.add), r=[cvec], w=[cvec])
                dve(lambda e: e.tensor_tensor(cvec.t[:, 6, :], cvec.t[:, 6, :], cvec.t[:, 4, :], op=ALU.mult), r=[cvec], w=[cvec])
                dve(lambda e: e.tensor_scalar(cvec.t[:, 4, :], cvec.t[:, 6, :], -8.0, None, op0=ALU.mult), r=[cvec], w=[cvec])
                dve(lambda e: e.tensor_scalar(cvec.t[:, 5, :], cvec.t[:, 6, :], -16.0, None, op0=ALU.mult), r=[cvec], w=[cvec])
                load_cast_rows(stg, stgsems, w_in, lambda kc: c_w_in[0, kc * P:(kc + 1) * P, :], KD, 2048, gmix)
                load_cast_rows(stg, stgsems, w_out, lambda kc: c_w_out[0, kc * P:(kc + 1) * P, :], KD, D, None)
                S.barrier()
                tmpsc.close()
                RW = route_bufs(sc, 1, wsem, MT3 // P)
                junk = RW[5]
                dve(lambda e: e.tensor_scalar(cvec.t[:, 6, :], cvec.t[:, 1, :], 0.5, None, op0=ALU.mult), r=[cvec], w=[cvec])
                dve(lambda e: e.tensor_scalar(cvec.t[:, 7, :], cvec.t[:, 2, :], 0.5, None, op0=ALU.mult), r=[cvec], w=[cvec])
                dve(lambda e: e.tensor_scalar(cvec.t[:, 3, :], cvec.t[:, 4, :], 0.5, None, op0=ALU.mult), r=[cvec], w=[cvec])
                NJ = MT3 // P
                NM = N // MT3
                xt_l = [sb(f"xt3{i}", [P, NJ, D], F32, sc) for i in range(3)]
                xsem = [S.newsem() for _ in range(3)]
                stsem = [S.newsem() for _ in range(3)]
                yg_l = [sb(f"yg{i}", [P, 2, D], BF16, sc) for i in range(4)]
                ygsem = [S.newsem() for _ in range(4)]
                hb_l = [sb(f"hb3{i}", [P, D], BF16, sc) for i in range(2)]
                hT = sb("hT3", [P, KD, MT3], BF16, sc)
                ss = sb("ss3", [P, NJ], F32, sc)
                rstd = sb("rstd3", [P, NJ], F32, sc)
                gg_l = [sb(f"gg{i}", [P, KD, MT3], BF16, sc) for i in range(2)]
                xr = sb("xr", [P, KD, MT3 + 3], F32, sc)
                xcf_l = [sb(f"xcf{i}", [P, MT3], F32, sc) for i in range(2)]
                XCB_l = [sb(f"XCB{i}", [P, KD, MT3], BF16, sc) for i in range(2)]
                TR = sb("TR", [P, KD, MT3], F32, sc)
                TI = sb("TI", [P, KD, MT3], F32, sc)
                AA = sb("AA", [P, KD, MT3], F32, sc)
                A2 = sb("A2", [P, KD, MT3], F32, sc)
                HS = A2
                hstate = sb("hstate", [P, KD], F32, sc)
                yT_l = [sb(f"yT3{i}", [P, KD, MT3], BF16, sc) for i in range(2)]
                pool(lambda e: e.memset(xr.t[:], 0.0), w=[xr])
                pool(lambda e: e.memset(hstate.t[:], 0.0), w=[hstate])

                def cb(nm):
                    return [Buf(f"{nm}{c}") for c in range(KD)]
                xr_c, TR_c, TI_c, AA_c, A2_c = cb("xr"), cb("TR"), cb("TI"), cb("AA"), cb("A2")
                gg_c = [cb("gga"), cb("ggb")]
                XCB_c = [cb("XCBa"), cb("XCBb")]
                yT_c = [cb("yTa"), cb("yTb")]
                S.barrier()
                zrot = [0]

                def zchunk(col0):
                    pz = ps[zrot[0] % 2]
                    zrot[0] += 1
                    for kc in range(KD):
                        pe(lambda e, kc=kc: e.matmul(pz.t[:, 0:MT3], w_in.t[:, kc, col0:col0 + P], hT.t[:, kc, :], start=(kc == 0), stop=(kc == KD - 1)),
                           r=[w_in, hT], w=[pz])
                    return pz

                def emit_load(m):
                    xt = xt_l[m % 3]
                    S.dma("sp", xsem[m % 3], lambda e: e.dma_start(out=xt.t[:], in_=xres_d[m * MT3:(m + 1) * MT3, :].rearrange("(j p) d -> p j d", p=P)),
                          r=[xres_b], w=[xt])
                    for j in range(NJ):
                        tile = m * NJ + j
                        yg = yg_l[tile % 4]
                        for k in range(2):
                            S.dma("pool", ygsem[tile % 4], lambda e, k=k, yg=yg, tile=tile: e.indirect_dma_start(
                                out=yg.t[:, k, :], out_offset=None, in_=ys_d[:, :],
                                in_offset=bass.IndirectOffsetOnAxis(ap=tokidx.t[:, tile, k:k + 1], axis=0)), r=[ys_b, tokidx], w=[yg], serial=True)

                def stageH(m):
                    xt = xt_l[m % 3]
                    sl = m % 2
                    gg, XCB = gg_l[sl], XCB_l[sl]
                    for j in range(NJ):
                        tile = m * NJ + j
                        yg = yg_l[tile % 4]
                        for k in range(2):
                            dve(lambda e, k=k, yg=yg, j=j, tile=tile: e.scalar_tensor_tensor(out=xt.t[:, j, :], in0=yg.t[:, k, :], scalar=tokinfo.t[:, tile, 2 + k:3 + k],
                                                                                         in1=xt.t[:, j, :], op0=ALU.mult, op1=ALU.add), r=[yg, tokinfo, xt], w=[xt])
                    yield
                    norm_and_transpose(sc, xt, NJ, hb_l, hT, ss, rstd, junk, ps[3])
                    yield
                    for c in range(KD):
                        pz = zchunk(c * P)
                        act(lambda e, c=c, pz=pz: e.activation(out=gg.t[:, c, :], in_=pz.t[:, 0:MT3], func=AF.Gelu_apprx_tanh), r=[pz], w=[gg_c[sl][c]])
                        if c % 2 == 1:
                            yield
                    for c in range(KD):
                        pz = zchunk(D + c * P)
                        act(lambda e, c=c, pz=pz: e.activation(out=xr.t[:, c, 3:MT3 + 3], in_=pz.t[:, 0:MT3], func=AF.Copy), r=[pz], w=[xr_c[c]])
                        if c % 2 == 1:
                            yield
                    for c in range(KD):
                        xcf = xcf_l[c % 2]
                        act(lambda e, c=c, xcf=xcf: e.activation(out=xcf.t[:], in_=xr.t[:, c, 3:MT3 + 3], func=AF.Identity, scale=cw.t[:, c, 3:4], bias=cvec.t[:, 0, c:c + 1]),
                            r=[xr_c[c], cw, cvec], w=[xcf])
                        for k in range(2):
                            dve(lambda e, c=c, k=k, xcf=xcf: e.scalar_tensor_tensor(out=xcf.t[:], in0=xr.t[:, c, k:MT3 + k], scalar=cw.t[:, c, k:k + 1], in1=xcf.t[:],
                                                                                  op0=ALU.mult, op1=ALU.add), r=[xr_c[c], cw, xcf], w=[xcf])
                        dve(lambda e, c=c, xcf=xcf: e.scalar_tensor_tensor(out=XCB.t[:, c, :], in0=xr.t[:, c, 2:MT3 + 2], scalar=cw.t[:, c, 2:3], in1=xcf.t[:],
                                                                          op0=ALU.mult, op1=ALU.add), r=[xr_c[c], cw, xcf], w=[XCB_c[sl][c]])
                        if c % 2 == 1:
                            yield
                    pool(lambda e: e.tensor_copy(xr.t[:, :, 0:3], xr.t[:, :, MT3:MT3 + 3]), r=xr_c, w=xr_c)
                    yield

                def stageE(m):
                    sl = m % 2
                    gg, XCB, yT = gg_l[sl], XCB_l[sl], yT_l[sl]
                    for c in range(KD):
                        pr, pi = ps[4], ps[5]
                        pe(lambda e, c=c, pr=pr: e.matmul(pr.t[:, 0:MT3], wa.t[:, c, :], XCB.t[:, c, :], start=True, stop=True), r=[wa, XCB_c[sl][c]], w=[pr])
                        pe(lambda e, c=c, pi=pi: e.matmul(pi.t[:, 0:MT3], wx.t[:, c, :], XCB.t[:, c, :], start=True, stop=True), r=[wx, XCB_c[sl][c]], w=[pi])
                        act(lambda e, c=c, pr=pr: e.activation(out=TR.t[:, c, :], in_=pr.t[:, 0:MT3], func=AF.Tanh, scale=0.5, bias=cvec.t[:, 6, c:c + 1]), r=[pr, cvec], w=[TR_c[c]])
                        act(lambda e, c=c, pi=pi: e.activation(out=TI.t[:, c, :], in_=pi.t[:, 0:MT3], func=AF.Tanh, scale=0.5, bias=cvec.t[:, 7, c:c + 1]), r=[pi, cvec], w=[TI_c[c]])
                        if c % 2 == 1:
                            yield
                    for c in range(KD):
                        act(lambda e, c=c: e.activation(out=AA.t[:, c, :], in_=TR.t[:, c, :], func=AF.Exp, scale=cvec.t[:, 3, c:c + 1], bias=cvec.t[:, 3, c:c + 1]), r=[TR_c[c], cvec], w=[AA_c[c]])
                        act(lambda e, c=c: e.activation(out=A2.t[:, c, :], in_=TR.t[:, c, :], func=AF.Exp, scale=cvec.t[:, 4, c:c + 1], bias=cvec.t[:, 4, c:c + 1]), r=[TR_c[c], cvec], w=[A2_c[c]])
                        if c % 4 == 3:
                            yield
                    for c in range(KD):
                        dve(lambda e, c=c: e.scalar_tensor_tensor(out=TI.t[:, c, :], in0=TI.t[:, c, :], scalar=1.0, in1=XCB.t[:, c, :], op0=ALU.add, op1=ALU.mult),
                            r=[TI_c[c], XCB_c[sl][c]], w=[TI_c[c]])
                    yield
                    for c in range(KD):
                        act(lambda e, c=c: e.activation(out=A2.t[:, c, :], in_=A2.t[:, c, :], func=AF.Relu, scale=-1.0, bias=1.0), r=[A2_c[c]], w=[A2_c[c]])
                    yield
                    for c in range(KD):
                        act(lambda e, c=c: e.activation(out=A2.t[:, c, :], in_=A2.t[:, c, :], func=AF.Sqrt), r=[A2_c[c]], w=[A2_c[c]])
                    yield
                    for c in range(KD):
                        dve(lambda e, c=c: e.scalar_tensor_tensor(out=TR.t[:, c, :], in0=A2.t[:, c, :], scalar=0.5, in1=TI.t[:, c, :], op0=ALU.mult, op1=ALU.mult),
                            r=[A2_c[c], TI_c[c]], w=[TR_c[c]])
                    yield
                    for c in range(KD):
                        dve(lambda e, c=c: e.tensor_tensor_scan(HS.t[:, c, :], AA.t[:, c, :], TR.t[:, c, :], hstate.t[:, c:c + 1], op0=ALU.mult, op1=ALU.add),
                            r=[AA_c[c], TR_c[c], hstate], w=[A2_c[c]])
                        if c % 4 == 3:
                            yield
                    dve(lambda e: e.tensor_copy(hstate.t[:, :], HS.t[:, :, MT3 - 1]), r=A2_c, w=[hstate])
                    for c in range(KD):
                        dve(lambda e, c=c: e.tensor_tensor(yT.t[:, c, :], gg.t[:, c, :], HS.t[:, c, :], op=ALU.mult), r=[gg_c[sl][c], A2_c[c]], w=[yT_c[sl][c]])
                    yield

                def stageB(m):
                    xt = xt_l[m % 3]
                    sl = m % 2
                    yT = yT_l[sl]
                    for j in range(NJ):
                        for half in range(2):
                            for c in range(KD):
                                pe(lambda e, c=c, half=half, j=j: e.matmul(psO.t[:, half * 512:(half + 1) * 512], yT.t[:, c, j * P:(j + 1) * P],
                                                                          w_out.t[:, c, half * 512:(half + 1) * 512], start=(c == 0), stop=(c == KD - 1)),
                                   r=[yT_c[sl][c], w_out], w=[psO])
                        dve(lambda e, j=j, xt=xt: e.tensor_tensor(xt.t[:, j, :], xt.t[:, j, :], psO.t[:], op=ALU.add), r=[xt, psO], w=[xt])
                        yield
                    yield from route_batch(1, xt, m * NJ, NJ, RW)
                    S.dma("sp", stsem[m % 3], lambda e, m=m, xt=xt: e.dma_start(out=xres_d[m * MT3:(m + 1) * MT3, :].rearrange("(j p) d -> p j d", p=P), in_=xt.t[:]),
                          r=[xt], w=[])
                    if m + 3 < NM:
                        emit_load(m + 3)

                emit_load(0)
                if NM > 1:
                    emit_load(1)
                for k in range(NM + 2):
                    interleave(stageH(k) if k < NM else None,
                               stageE(k - 1) if 0 <= k - 1 < NM else None,
                               stageB(k - 2) if 0 <= k - 2 < NM else None)
                    if k == 0 and NM > 2:
                        emit_load(2)
                flush_scatters()
                S.barrier()

        def pass5(src_d, do_combine, do_norm):
            with ExitStack() as sc:
                curscope[0] = sc
                NJ = 4
                MT = NJ * P
                NM = N // MT
                xt_l = [sb(f"xt5{i}", [P, NJ, D], F32, sc) for i in range(2)]
                xsem = [S.newsem(), S.newsem()]
                stsem = [S.newsem(), S.newsem()]
                yg_l = [sb(f"yg5{i}", [P, 2, D], BF16, sc) for i in range(8)]
                ygsem = [S.newsem() for _ in range(8)]
                junk = sb("junk5", [P, D], BF16, sc)
                ss = sb("ss5", [P, NJ], F32, sc)
                rstd = sb("rstd5", [P, NJ], F32, sc)

                def load5(m):
                    xt = xt_l[m % 2]
                    S.dma("sp", xsem[m % 2], lambda e: e.dma_start(out=xt.t[:], in_=src_d[m * MT:(m + 1) * MT, :].rearrange("(j p) d -> p j d", p=P)),
                          r=[xres_b], w=[xt])
                    if do_combine:
                        for j in range(NJ):
                            tile = m * NJ + j
                            yg = yg_l[tile % 8]
                            for k in range(2):
                                S.dma("pool", ygsem[tile % 8], lambda e, k=k, yg=yg, tile=tile: e.indirect_dma_start(
                                    out=yg.t[:, k, :], out_offset=None, in_=ys_d[:, :],
                                    in_offset=bass.IndirectOffsetOnAxis(ap=tokidx.t[:, tile, k:k + 1], axis=0)), r=[ys_b, tokidx], w=[yg], serial=True)

                ss_l = [ss, sb("ss5b", [P, NJ], F32, sc)]
                rstd_l = [rstd, sb("rstd5b", [P, NJ], F32, sc)]

                def s1(m):
                    xt = xt_l[m % 2]
                    ssm, rstdm = ss_l[m % 2], rstd_l[m % 2]
                    for j in range(NJ):
                        tile = m * NJ + j
                        if do_combine:
                            yg = yg_l[tile % 8]
                            for k in range(2):
                                dve(lambda e, k=k, yg=yg, j=j, tile=tile: e.scalar_tensor_tensor(out=xt.t[:, j, :], in0=yg.t[:, k, :], scalar=tokinfo.t[:, tile, 2 + k:3 + k],
                                                                                             in1=xt.t[:, j, :], op0=ALU.mult, op1=ALU.add), r=[yg, tokinfo, xt], w=[xt])
                        if do_norm:
                            act(lambda e, j=j, xt=xt: e.activation(out=junk.t[:], in_=xt.t[:, j, :], func=AF.Square, accum_out=ssm.t[:, j:j + 1]), r=[xt], w=[junk, ssm])
                        yield
                    if do_norm:
                        rstd_from_ss(rstdm, ssm, D, NJ)
                    yield

                def s2(m):
                    xt = xt_l[m % 2]
                    rstdm = rstd_l[m % 2]
                    if do_norm:
                        for j in range(NJ):
                            dve(lambda e, j=j, xt=xt: e.scalar_tensor_tensor(out=xt.t[:, j, :], in0=xt.t[:, j, :], scalar=rstdm.t[:, j:j + 1], in1=gfin_rep.t[:],
                                                                             op0=ALU.mult, op1=ALU.mult), r=[xt, rstdm, gfin_rep], w=[xt])
                            yield
                    S.dma("sp", stsem[m % 2], lambda e, m=m, xt=xt: e.dma_start(out=out_d[m * MT:(m + 1) * MT, :].rearrange("(j p) d -> p j d", p=P), in_=xt.t[:]),
                          r=[xt], w=[])
                    if m + 2 < NM:
                        load5(m + 2)
                    yield

                load5(0)
                if NM > 1:
                    load5(1)
                interleave(s1(0))
                for m in range(NM):
                    interleave(s1(m + 1) if m + 1 < NM else None, s2(m))
                S.dma("sp", stsem[0], lambda e: e.dma_start(out=dbg_d, in_=tokinfo.t[:].rearrange("p t f -> p (t f)")), r=[tokinfo], w=[])
                S.barrier()

        pass1()
        if stop_after >= 2:
            pass_experts(0)
        if stop_after >= 3:
            pass3()
        if stop_after >= 4:
            pass_experts(1)
        if stop_after >= 5:
            pass5(xres_d, True, True)
        elif stop_after in (2, 4):
            pass5(xres_d, True, False)
        else:
            pass5(xres_d, False, False)
    global LAST_DUMPS
    LAST_DUMPS = dumps
    return nc


WKEYS = ["norm_mix_g", "norm_ffn_g", "norm_final_g", "ab_w_in", "a_ln_g", "a_ws", "a_ws_b", "b_conv_w", "ab_w_out",
         "c_w_in", "c_conv_w", "c_conv_b", "c_w_a", "c_b_a", "c_w_x", "c_b_x", "c_lambda", "c_w_out",
         "moe_w_rg", "moe_b_rg", "moe_w_re", "moe_b_re", "moe_w_gate", "moe_w_up", "moe_w_down"]

CAP_FULL = 1024


def kernel(**inputs):
    x = np.ascontiguousarray(inputs["x"], dtype=np.float32)
    B, T, _ = x.shape
    nc = build(T, CAP_FULL)
    w = {k: np.ascontiguousarray(inputs[k], dtype=np.float32) for k in WKEYS}
    in_maps = []
    for b in range(B):
        m = dict(w)
        m["x"] = x[b]
        in_maps.append(m)
    res = run_bass_kernel_spmd(nc, in_maps, core_ids=list(range(B)))
    return np.stack([np.asarray(r["out"], dtype=np.float32) for r in res.results], axis=0)
```

```python
import numpy as np
from contextlib import ExitStack
import concourse.bass as bass
import concourse.mybir as mybir
from concourse.bass_utils import run_bass_kernel_spmd

F32 = mybir.dt.float32
BF16 = mybir.dt.bfloat16
I32 = mybir.dt.int32
AF = mybir.ActivationFunctionType
ALU = mybir.AluOpType
AX = mybir.AxisListType

SETTLE = False
SERIAL_IND = False
P = 128
D = 1024
KD = 8
NE = 32
EPS = 1e-6


class Sem:
    def __init__(self, h, owner=None):
        self.h = h
        self.val = 0
        self.owner = owner


class Buf:
    def __init__(self, name):
        self.name = name
        self.w = {}
        self.r = {}


class T:
    def __init__(self, t, name):
        self.t = t
        self.b = Buf(name)


class Sch:
    def __init__(self, nc, es):
        self.nc = nc
        self.es = es
        self.engs = {"pe": nc.tensor, "act": nc.scalar, "dve": nc.vector, "pool": nc.gpsimd, "sp": nc.sync}
        self.esem = {}
        self.known = {e: {} for e in self.engs}
        self.dsems = []
        self.nsem = 0
        for e in self.engs:
            self.esem[e] = self.newsem(e)
        self.scratch = {e: es.enter_context(nc.sbuf_tensor(f"scr_{e}", [P, 2], F32)) for e in ("act", "dve", "pool")}
        nc.vector.memset(self.scratch["act"][:], 0.0)

    def newsem(self, owner=None):
        self.nsem += 1
        h = self.es.enter_context(self.nc.semaphore(f"s{self.nsem}"))
        s = Sem(h, owner)
        if owner is None:
            self.dsems.append(s)
        return s

    def _waits(self, eng, reads, writes):
        need = {}
        for b in reads:
            for s, v in b.w.items():
                if need.get(s, 0) < v:
                    need[s] = v
        for b in writes:
            for dct in (b.w, b.r):
                for s, v in dct.items():
                    if need.get(s, 0) < v:
                        need[s] = v
        E = self.engs[eng]
        kn = self.known[eng]
        for s, v in need.items():
            if s.owner == eng and eng == "pe":
                continue
            if SETTLE and s.owner in ("act", "dve", "pool") and s.owner != eng:
                v = v + 1
                if s.val < v:
                    self._dummy(s.owner)
            if kn.get(s, 0) >= v:
                continue
            E.wait_ge(s.h, v)
            kn[s] = v

    def _dummy(self, eng):
        E = self.engs[eng]
        t = self.scratch[eng]
        if eng == "act":
            ins = E.activation(out=t[:, 1:2], in_=t[:, 0:1], func=AF.Copy)
        else:
            ins = E.memset(t[:, 0:1], 0.0)
        s = self.esem[eng]
        s.val += 1
        ins.then_inc(s.h, 1)

    def _mark(self, s, v, reads, writes):
        for b in reads:
            if b.r.get(s, 0) < v:
                b.r[s] = v
        for b in writes:
            if b.w.get(s, 0) < v:
                b.w[s] = v

    def op(self, eng, fn, r=(), w=()):
        reads, writes = r, w
        reads = [x.b if isinstance(x, T) else x for x in reads]
        writes = [x.b if isinstance(x, T) else x for x in writes]
        self._waits(eng, reads, writes)
        ins = fn(self.engs[eng])
        s = self.esem[eng]
        s.val += 1
        ins.then_inc(s.h, 1)
        self._mark(s, s.val, reads, writes)

    def dma(self, q, sem, fn, r=(), w=(), serial=False, defer=None):
        reads, writes = r, w
        reads = [x.b if isinstance(x, T) else x for x in reads]
        writes = [x.b if isinstance(x, T) else x for x in writes]
        self._waits(q, reads, writes)
        if serial and SERIAL_IND:
            last = getattr(self, "last_ind", None)
            if last is not None and self.known[q].get(last[0], 0) < last[1]:
                self.engs[q].wait_ge(last[0].h, last[1])
                self.known[q][last[0]] = last[1]
        ins = fn(self.engs[q])
        sem.val += 16
        ins.then_inc(sem.h, 16)
        if serial:
            self.last_ind = (sem, sem.val)
        if defer is not None:
            defer.append((sem, writes))
            self._mark(sem, sem.val, reads, [])
        else:
            self._mark(sem, sem.val, reads, writes)

    def flush(self, lst):
        for sem, writes in lst:
            self._mark(sem, sem.val, [], writes)
        del lst[:]

    def barrier(self, skip=()):
        allsems = [x for x in list(self.esem.values()) + self.dsems if x not in skip]
        for e, E in self.engs.items():
            kn = self.known[e]
            for s in allsems:
                if s.owner == e or s.val == 0:
                    continue
                if kn.get(s, 0) >= s.val:
                    continue
                E.wait_ge(s.h, s.val)
                kn[s] = s.val


def interleave(*gens):
    gens = [g for g in gens if g is not None]
    while gens:
        for g in list(gens):
            try:
                next(g)
            except StopIteration:
                gens.remove(g)


def build(N, CAP, stop_after=5, MT1=512, MT3=256, debug=False):
    NT = N // P
    NSLOT = NE * CAP
    nc = bass.Bass("TRN2", target_bir_lowering=False)

    def din(name, shape, dt=F32):
        return nc.dram_tensor(name, list(shape), dt, kind="ExternalInput").ap()

    x_d = din("x", [N, D])
    norm_mix_g = din("norm_mix_g", [2, D])
    norm_ffn_g = din("norm_ffn_g", [2, D])
    norm_final_g = din("norm_final_g", [D])
    ab_w_in = din("ab_w_in", [1, D, 2560])
    a_ln_g = din("a_ln_g", [1, 512])
    a_ws = din("a_ws", [1, 4, 128, 128])
    a_ws_b = din("a_ws_b", [1, 4, 128])
    b_conv_w = din("b_conv_w", [1, 3, 512])
    ab_w_out = din("ab_w_out", [1, D, D])
    c_w_in = din("c_w_in", [1, D, 2048])
    c_conv_w = din("c_conv_w", [1, 4, D])
    c_conv_b = din("c_conv_b", [1, D])
    c_w_a = din("c_w_a", [1, 8, 128, 128])
    c_b_a = din("c_b_a", [1, D])
    c_w_x = din("c_w_x", [1, 8, 128, 128])
    c_b_x = din("c_b_x", [1, D])
    c_lambda = din("c_lambda", [1, D])
    c_w_out = din("c_w_out", [1, D, D])
    moe_w_rg = din("moe_w_rg", [2, D, 4])
    moe_b_rg = din("moe_b_rg", [2, 4])
    moe_w_re = din("moe_w_re", [2, D, 32])
    moe_b_re = din("moe_b_re", [2, 32])
    moe_w_gate = din("moe_w_gate", [2, NE, D, 512])
    moe_w_up = din("moe_w_up", [2, NE, D, 512])
    moe_w_down = din("moe_w_down", [2, NE, 512, D])
    out_d = nc.dram_tensor("out", [N, D], F32, kind="ExternalOutput").ap()
    dbg_d = nc.dram_tensor("dbg", [P, NT * 4], F32, kind="ExternalOutput").ap()
    dbg2_d = nc.dram_tensor("dbg2", [P, 16384], F32, kind="ExternalOutput").ap() if debug else None
    dumps = {}
    xres_d = nc.dram_tensor("xres", [N, D], F32, kind="Internal").ap()
    xs_d = nc.dram_tensor("xs", [NSLOT, D], BF16, kind="Internal").ap()
    ys_d = nc.dram_tensor("ys", [NSLOT, D], BF16, kind="Internal").ap()
    xres_b = Buf("xres")
    xs_b = Buf("xs")
    xs_zero = Buf("xs_zero")
    ys_b = Buf("ys")

    with ExitStack() as es:
        S = Sch(nc, es)

        sbn = [0]

        def sb(name, shape, dt, scope=es):
            sbn[0] += 1
            name = f"{name}_{sbn[0]}"
            return T(scope.enter_context(nc.sbuf_tensor(name, list(shape), dt)), name)

        def act(fn, r=(), w=()):
            S.op("act", fn, r, w)

        def dve(fn, r=(), w=()):
            S.op("dve", fn, r, w)

        def pool(fn, r=(), w=()):
            S.op("pool", fn, r, w)

        def pe(fn, r=(), w=()):
            S.op("pe", fn, r, w)

        dcol = [0]
        curscope = [es]

        def dump(name, ap, width, rd):
            if not debug or name in dumps:
                return
            stt = sb("dmp_" + name, [P, width], F32, curscope[0])
            dsem = S.newsem()
            dve(lambda e: e.tensor_copy(stt.t[0:ap.shape[0], :], ap), r=rd, w=[stt])
            c0 = dcol[0]
            S.dma("sp", dsem, lambda e: e.dma_start(out=dbg2_d[0:ap.shape[0], c0:c0 + width], in_=stt.t[0:ap.shape[0], :]), r=[stt], w=[])
            dumps[name] = (c0, width)
            dcol[0] += width

        ps = [T(es.enter_context(nc.psum_tensor(f"ps{i}", [P, 512], F32)), f"ps{i}") for i in range(6)]
        psO = T(es.enter_context(nc.psum_tensor("psO", [P, 1024], F32)), "psO")

        iot = sb("iot", [P, P], I32)
        ident_f = sb("ident_f", [P, P], F32)
        ident_b = sb("ident_b", [P, P], BF16)
        tri_f = sb("tri_f", [P, P], F32)
        ones_row_f = sb("ones_row_f", [1, P], F32)
        ones_row_b = sb("ones_row_b", [1, P], BF16)
        ones_col_f = sb("ones_col_f", [P, 1], F32)
        econst = sb("econst", [P, 16], F32)
        ones_mat = sb("ones_mat", [P, P], F32)
        ebaseN = sb("ebaseN", [P, 4 * NE], F32)
        mhalf = sb("mhalf", [P, 8], F32)
        ebase_i = sb("ebase_i", [P, NE], I32)
        ebase = sb("ebase", [P, NE], F32)
        base_row = sb("base_row", [1, NE], F32)
        tokinfo = sb("tokinfo", [P, NT, 4], F32)
        tokidx = sb("tokidx", [P, NT, 2], I32)
        gfin_rep = sb("gfin_rep", [P, D], F32)
        csem = S.newsem()

        pool(lambda e: e.iota(iot.t[:], pattern=[[1, P]], base=0, channel_multiplier=-1), w=[iot])
        dve(lambda e: e.tensor_scalar(ident_f.t[:], iot.t[:], 0.0, None, op0=ALU.is_equal), r=[iot], w=[ident_f])
        dve(lambda e: e.tensor_scalar(tri_f.t[:], iot.t[:], 0.0, None, op0=ALU.is_ge), r=[iot], w=[tri_f])
        dve(lambda e: e.tensor_copy(ident_b.t[:], ident_f.t[:]), r=[ident_f], w=[ident_b])
        pool(lambda e: e.memset(ones_row_f.t[:], 1.0), w=[ones_row_f])
        pool(lambda e: e.memset(ones_row_b.t[:], 1.0), w=[ones_row_b])
        pool(lambda e: e.memset(ones_col_f.t[:], 1.0), w=[ones_col_f])
        pool(lambda e: e.memset(econst.t[:], float(np.e)), w=[econst])
        pool(lambda e: e.memset(mhalf.t[:], -0.5), w=[mhalf])
        pool(lambda e: e.iota(ebase_i.t[:], pattern=[[CAP, NE]], base=0, channel_multiplier=0), w=[ebase_i])
        dve(lambda e: e.tensor_copy(ebase.t[:], ebase_i.t[:]), r=[ebase_i], w=[ebase])
        for j_ in range(4):
            dve(lambda e, j_=j_: e.tensor_copy(ebaseN.t[:, j_ * NE:(j_ + 1) * NE], ebase_i.t[:]), r=[ebase_i], w=[ebaseN])
        dve(lambda e: e.memset(ones_mat.t[:], 1.0), w=[ones_mat])
        S.dma("sp", csem, lambda e: e.dma_start(out=gfin_rep.t[:], in_=norm_final_g.partition_broadcast(P)), w=[gfin_rep])

        bound_reg = nc.gpsimd.to_reg(NSLOT - 1)

        def rstd_from_ss(rstd, ss, n, cols):
            pool(lambda e: e.tensor_scalar(rstd.t[:, 0:cols], ss.t[:, 0:cols], 1.0 / n, EPS, op0=ALU.mult, op1=ALU.add),
                 r=[ss], w=[rstd])
            pool(lambda e: e.tensor_tensor(rstd.t[:, 0:cols], rstd.t[:, 0:cols], mhalf.t[:, 0:cols], op=ALU.pow),
                 r=[rstd, mhalf], w=[rstd])

        def load_cast_rows(scope_stg, stgsems, dst, dram_rows_fn, nk, width, gcol=None, cnt=[0]):
            for kc in range(nk):
                i = cnt[0] % 2
                cnt[0] += 1
                st = scope_stg[i]
                S.dma("sp", stgsems[i], lambda e, kc=kc, st=st: e.dma_start(out=st.t[:, 0:width], in_=dram_rows_fn(kc)), w=[st])
                if gcol is not None:
                    if kc % 2 == 0:
                        dve(lambda e, kc=kc, st=st: e.tensor_scalar(dst.t[:, kc, :], st.t[:, 0:width], gcol.t[:, kc:kc + 1], None, op0=ALU.mult),
                            r=[st, gcol], w=[dst])
                    else:
                        act(lambda e, kc=kc, st=st: e.activation(out=dst.t[:, kc, :], in_=st.t[:, 0:width], func=AF.Identity, scale=gcol.t[:, kc:kc + 1]),
                            r=[st, gcol], w=[dst])
                else:
                    if kc % 2 == 0:
                        dve(lambda e, kc=kc, st=st: e.tensor_copy(dst.t[:, kc, :], st.t[:, 0:width]), r=[st], w=[dst])
                    else:
                        act(lambda e, kc=kc, st=st: e.activation(out=dst.t[:, kc, :], in_=st.t[:, 0:width], func=AF.Copy), r=[st], w=[dst])

        def norm_and_transpose(sc, xt, nj, hb_l, hT, ss, rstd, junk, PT):
            for j in range(nj):
                act(lambda e, j=j: e.activation(out=junk.t[:], in_=xt.t[:, j, :], func=AF.Square, accum_out=ss.t[:, j:j + 1]),
                    r=[xt], w=[junk, ss])
            rstd_from_ss(rstd, ss, D, nj)
            dump("ss", ss.t[:, 0:nj], nj, [ss])
            dump("rstd", rstd.t[:, 0:nj], nj, [rstd])
            for j in range(nj):
                hb = hb_l[j % 2]
                dve(lambda e, j=j, hb=hb: e.tensor_scalar(hb.t[:], xt.t[:, j, :], rstd.t[:, j:j + 1], None, op0=ALU.mult),
                    r=[xt, rstd], w=[hb])
                pass
                ptv = PT.t[:].bitcast(BF16)
                for kc in range(KD):
                    pe(lambda e, kc=kc, hb=hb: e.transpose(ptv[:, kc * P:(kc + 1) * P], hb.t[:, kc * P:(kc + 1) * P], ident_b.t[:]),
                       r=[hb, ident_b], w=[PT])
                act(lambda e, j=j: e.activation(out=hT.t[:, :, j * P:(j + 1) * P], in_=ptv.rearrange("p (k t) -> p k t", k=KD), func=AF.Copy),
                    r=[PT], w=[hT])
                dump("hT0", hT.t[:, 0, 0:P], P, [hT])

        def ffn_norm_route_scatter(sc, lyr, xt, j, tile, W):
            (gffn_rep, wr, br_row, ss2, rstd2, junk, h2g_l, h2b_l, h2T, PSR, lg, rt, scat_sems) = W
            h2g = h2g_l[tile % 2]
            h2b = h2b_l[tile % 2]
            act(lambda e: e.activation(out=junk.t[:], in_=xt.t[:, j, :], func=AF.Square, accum_out=ss2.t[:, 0:1]), r=[xt], w=[junk, ss2])
            rstd_from_ss(rstd2, ss2, D, 1)
            dve(lambda e: e.scalar_tensor_tensor(out=h2g.t[:], in0=xt.t[:, j, :], scalar=rstd2.t[:, 0:1], in1=gffn_rep.t[:],
                                                 op0=ALU.mult, op1=ALU.mult), r=[xt, rstd2, gffn_rep], w=[h2g])
            act(lambda e: e.activation(out=h2b.t[:], in_=h2g.t[:], func=AF.Copy), r=[h2g], w=[h2b])
            for kc in range(KD):
                pe(lambda e, kc=kc: e.transpose(psO.t[:, kc * P:(kc + 1) * P], h2g.t[:, kc * P:(kc + 1) * P], ident_f.t[:]),
                   r=[h2g, ident_f], w=[psO])
            dve(lambda e: e.tensor_copy(h2T.t[:], psO.t[:].rearrange("p (k t) -> p k t", k=KD)), r=[psO], w=[h2T])
            for kc in range(KD):
                pe(lambda e, kc=kc: e.matmul(PSR.t[:, 0:36], h2T.t[:, kc, :], wr.t[:, kc, :], start=(kc == 0), stop=False),
                   r=[h2T, wr], w=[PSR])
            pe(lambda e: e.matmul(PSR.t[:, 0:36], ones_row_f.t[0:1, :], br_row.t[0:1, :], start=False, stop=True),
               r=[ones_row_f, br_row], w=[PSR])
            dve(lambda e: e.tensor_copy(lg.t[:], PSR.t[:, 0:36]), r=[PSR], w=[lg])
            R = rt.t
            dve(lambda e: e.tensor_reduce(out=R[:, 0:1], in_=lg.t[:, 0:4], axis=AX.X, op=ALU.max), r=[lg], w=[rt])
            dve(lambda e: e.tensor_scalar(R[:, 1:5], lg.t[:, 0:4], R[:, 0:1], None, op0=ALU.is_ge), r=[lg, rt], w=[rt])
            dve(lambda e: e.tensor_scalar(R[:, 5:9], lg.t[:, 0:4], R[:, 0:1], None, op0=ALU.subtract), r=[lg, rt], w=[rt])
            pool(lambda e: e.tensor_tensor(R[:, 5:9], econst.t[:, 0:4], R[:, 5:9], op=ALU.pow), r=[rt, econst], w=[rt])
            dve(lambda e: e.tensor_reduce(out=R[:, 9:10], in_=R[:, 5:9], axis=AX.X, op=ALU.add), r=[rt], w=[rt])
            dve(lambda e: e.reciprocal(R[:, 10:11], R[:, 9:10]), r=[rt], w=[rt])
            dve(lambda e: e.tensor_scalar(R[:, 11:19], lg.t[:, 4:12], R[:, 1:2], None, op0=ALU.mult), r=[lg, rt], w=[rt])
            for g in range(1, 4):
                dve(lambda e, g=g: e.scalar_tensor_tensor(out=R[:, 11:19], in0=lg.t[:, 4 + 8 * g:12 + 8 * g], scalar=R[:, 1 + g:2 + g],
                                                          in1=R[:, 11:19], op0=ALU.mult, op1=ALU.add), r=[lg, rt], w=[rt])
            dve(lambda e: e.max(R[:, 19:27], R[:, 11:19]), r=[rt], w=[rt])
            dve(lambda e: e.tensor_scalar(R[:, 27:35], R[:, 11:19], R[:, 19:20], None, op0=ALU.is_ge), r=[rt], w=[rt])
            dve(lambda e: e.tensor_scalar(R[:, 35:43], R[:, 11:19], R[:, 20:21], None, op0=ALU.is_ge), r=[rt], w=[rt])
            dve(lambda e: e.tensor_tensor(R[:, 43:44], R[:, 20:21], R[:, 19:20], op=ALU.subtract), r=[rt], w=[rt])
            pool(lambda e: e.tensor_tensor(R[:, 44:45], econst.t[:, 0:1], R[:, 43:44], op=ALU.pow), r=[rt, econst], w=[rt])
            dve(lambda e: e.tensor_scalar(R[:, 45:46], R[:, 44:45], 1.0, None, op0=ALU.add), r=[rt], w=[rt])
            dve(lambda e: e.reciprocal(R[:, 45:46], R[:, 45:46]), r=[rt], w=[rt])
            dve(lambda e: e.tensor_tensor(R[:, 46:47], R[:, 44:45], R[:, 45:46], op=ALU.mult), r=[rt], w=[rt])
            dve(lambda e: e.tensor_scalar(R[:, 47:49], R[:, 45:47], R[:, 10:11], None, op0=ALU.mult), r=[rt], w=[rt])
            M12, M1, M2, pos = W_m = (rt.t[:, 64:96], rt.t[:, 96:128], rt.t[:, 128:160], rt.t[:, 160:192])
            for g in range(4):
                dve(lambda e, g=g: e.tensor_scalar(M12[:, 8 * g:8 * g + 8], R[:, 35:43], R[:, 1 + g:2 + g], None, op0=ALU.mult), r=[rt], w=[rt])
                dve(lambda e, g=g: e.tensor_scalar(M1[:, 8 * g:8 * g + 8], R[:, 27:35], R[:, 1 + g:2 + g], None, op0=ALU.mult), r=[rt], w=[rt])
            dve(lambda e: e.tensor_tensor(M2, M12, M1, op=ALU.subtract), r=[rt], w=[rt])
            pe(lambda e: e.matmul(PSR.t[:, 64:96], tri_f.t[:], M12, start=True, stop=False), r=[tri_f, rt], w=[PSR])
            pe(lambda e: e.matmul(PSR.t[:, 64:96], ones_row_f.t[0:1, :], base_row.t[0:1, :], start=False, stop=True),
               r=[ones_row_f, base_row], w=[PSR])
            pe(lambda e: e.matmul(PSR.t[0:1, 128:160], ones_col_f.t[:, 0:1], M12, start=True, stop=True), r=[ones_col_f, rt], w=[PSR])
            dve(lambda e: e.scalar_tensor_tensor(out=pos, in0=PSR.t[:, 64:96], scalar=-1.0, in1=ebase.t[:], op0=ALU.add, op1=ALU.add),
                r=[PSR, ebase], w=[rt])
            dve(lambda e: e.tensor_scalar(rt.t[:, 192:224], PSR.t[:, 64:96], float(CAP), float(NSLOT), op0=ALU.is_gt, op1=ALU.mult),
                r=[PSR], w=[rt])
            dve(lambda e: e.tensor_tensor(pos, pos, rt.t[:, 192:224], op=ALU.add), r=[rt], w=[rt])
            dve(lambda e: e.tensor_tensor(base_row.t[0:1, :], base_row.t[0:1, :], PSR.t[0:1, 128:160], op=ALU.add), r=[PSR, base_row], w=[base_row])
            dve(lambda e: e.tensor_tensor(rt.t[:, 224:256], M1, pos, op=ALU.mult), r=[rt], w=[rt])
            dve(lambda e: e.tensor_reduce(out=R[:, 49:50], in_=rt.t[:, 224:256], axis=AX.X, op=ALU.add), r=[rt], w=[rt])
            dve(lambda e: e.tensor_tensor(rt.t[:, 224:256], M2, pos, op=ALU.mult), r=[rt], w=[rt])
            dve(lambda e: e.tensor_reduce(out=R[:, 50:51], in_=rt.t[:, 224:256], axis=AX.X, op=ALU.add), r=[rt], w=[rt])
            dve(lambda e: e.tensor_scalar(R[:, 51:53], R[:, 49:51], float(NSLOT) - 0.5, None, op0=ALU.is_lt), r=[rt], w=[rt])
            dve(lambda e: e.tensor_tensor(tokinfo.t[:, tile, 2:4], R[:, 47:49], R[:, 51:53], op=ALU.mult), r=[rt], w=[tokinfo])
            dve(lambda e: e.tensor_scalar(tokinfo.t[:, tile, 0:2], R[:, 49:51], float(NSLOT - 1), None, op0=ALU.min), r=[rt], w=[tokinfo])
            dve(lambda e: e.tensor_copy(tokidx.t[:, tile, :], tokinfo.t[:, tile, 0:2]), r=[tokinfo], w=[tokidx])
            dump("lg", lg.t[:], 36, [lg])
            dump("rt", rt.t[:], 256, [rt])
            dump("h2T0", h2T.t[:, 0, :], P, [h2T])
            sidx = h2b_l[2 + tile % 2]
            dve(lambda e: e.tensor_copy(sidx.t[:], R[:, 49:51]), r=[rt], w=[sidx])
            for k in range(2):
                S.dma("pool", scat_sems[tile % 2],
                      lambda e, k=k: e.indirect_dma_start(out=xs_d[:, :], out_offset=bass.IndirectOffsetOnAxis(ap=sidx.t[:, k:k + 1], axis=0),
                                                          in_=h2b.t[:, :], in_offset=None, bounds_check=bound_reg, oob_is_err=False),
                      r=[h2b, sidx], w=[], serial=True)

        def router_weights(sc, lyr, sem):
            gffn_rep = sb(f"gffn_rep{lyr}", [P, D], F32, sc)
            wr = sb(f"wr{lyr}", [P, KD, 36], F32, sc)
            br_row = sb(f"br_row{lyr}", [1, 36], F32, sc)
            grp = []
            S.dma("sp", sem, lambda e: e.dma_start(out=gffn_rep.t[:], in_=norm_ffn_g[lyr].partition_broadcast(P)), w=[gffn_rep], defer=grp)
            S.dma("sp", sem, lambda e: e.dma_start(out=wr.t[:, :, 0:4], in_=moe_w_rg[lyr].rearrange("(k p) g -> p k g", p=P)), w=[wr], defer=grp)
            S.dma("sp", sem, lambda e: e.dma_start(out=wr.t[:, :, 4:36], in_=moe_w_re[lyr].rearrange("(k p) g -> p k g", p=P)), w=[wr], defer=grp)
            S.dma("sp", sem, lambda e: e.dma_start(out=br_row.t[0:1, 0:4], in_=moe_b_rg[lyr:lyr + 1, :]), w=[br_row], defer=grp)
            S.dma("sp", sem, lambda e: e.dma_start(out=br_row.t[0:1, 4:36], in_=moe_b_re[lyr:lyr + 1, :]), w=[br_row], defer=grp)
            S.flush(grp)
            return gffn_rep, wr, br_row

        def route_bufs(sc, lyr, sem, NJ):
            gffn_rep, wr, br_row = router_weights(sc, lyr, sem)
            ss2 = sb("ss2", [P, 4], F32, sc)
            rstd2 = sb("rstd2", [P, 4], F32, sc)
            junk = sb("junk", [P, D], BF16, sc)
            h2g_l = [sb(f"h2g{i}", [P, D], F32, sc) for i in range(2 if NJ == 4 else 1)]
            h2b_l = [sb(f"h2b{i}", [P, D], BF16, sc) for i in range(NJ)]
            sidx = sb("sidx", [P, 4, 2], I32, sc)
            h2T = sb("h2T", [P, KD, P], F32, sc)
            rt = sb("rt", [P, 1024], F32, sc)
            scat_sems = [S.newsem() for _ in range(NJ)]
            pool(lambda e: e.memset(base_row.t[:], 0.0), w=[base_row])
            return (gffn_rep, wr, br_row, ss2, rstd2, junk, h2g_l, h2b_l, h2T, ps[2], sidx, rt, scat_sems)

        pending_scatter = []

        def flush_scatters():
            while pending_scatter:
                pending_scatter.pop(0)()

        def route_batch(lyr, xt, tile0, NJ, W):
            (gffn_rep, wr, br_row, ss2, rstd2, junk, h2g_l, h2b_l, h2T, PSR, sidx, rt, scat_sems) = W
            R = rt.t
            for j in range(NJ):
                act(lambda e, j=j: e.activation(out=junk.t[:], in_=xt.t[:, j, :], func=AF.Square, accum_out=ss2.t[:, j:j + 1]), r=[xt], w=[junk, ss2])
            rstd_from_ss(rstd2, ss2, D, NJ)
            flush_scatters()
            for j in range(NJ):
                h2g = h2g_l[j % len(h2g_l)]
                h2b = h2b_l[j]
                dve(lambda e, j=j, h2g=h2g: e.scalar_tensor_tensor(out=h2g.t[:], in0=xt.t[:, j, :], scalar=rstd2.t[:, j:j + 1], in1=gffn_rep.t[:],
                                                                   op0=ALU.mult, op1=ALU.mult), r=[xt, rstd2, gffn_rep], w=[h2g])
                act(lambda e, h2g=h2g, h2b=h2b: e.activation(out=h2b.t[:], in_=h2g.t[:], func=AF.Copy), r=[h2g], w=[h2b])
                for kc in range(KD):
                    pe(lambda e, kc=kc, h2g=h2g: e.transpose(psO.t[:, kc * P:(kc + 1) * P], h2g.t[:, kc * P:(kc + 1) * P], ident_f.t[:]),
                       r=[h2g, ident_f], w=[psO])
                if j % 2 == 0:
                    dve(lambda e: e.tensor_copy(h2T.t[:], psO.t[:].rearrange("p (k t) -> p k t", k=KD)), r=[psO], w=[h2T])
                else:
                    act(lambda e: e.activation(out=h2T.t[:], in_=psO.t[:].rearrange("p (k t) -> p k t", k=KD), func=AF.Copy), r=[psO], w=[h2T])
                for kc in range(KD):
                    pe(lambda e, kc=kc, j=j: e.matmul(PSR.t[:, j * 36:(j + 1) * 36], h2T.t[:, kc, :], wr.t[:, kc, :], start=(kc == 0), stop=False),
                       r=[h2T, wr], w=[PSR])
                pe(lambda e, j=j: e.matmul(PSR.t[:, j * 36:(j + 1) * 36], ones_row_f.t[0:1, :], br_row.t[0:1, :], start=False, stop=True),
                   r=[ones_row_f, br_row], w=[PSR])
                yield
            J4, J8, J32 = NJ * 4, NJ * 8, NJ * 32
            lg3 = R[:, 0:NJ * 36].rearrange("p (j c) -> p j c", j=NJ)
            gmax = R[:, 144:144 + NJ]
            goh = R[:, 148:148 + J4].rearrange("p (j g) -> p j g", j=NJ)
            d4f = R[:, 164:164 + J4]
            d4 = d4f.rearrange("p (j g) -> p j g", j=NJ)
            gsum = R[:, 180:180 + NJ]
            gprob = R[:, 184:184 + NJ]
            prodf = R[:, 192:192 + J32]
            prod = prodf.rearrange("p (j g e) -> p j g e", j=NJ, g=4)
            esel = R[:, 320:320 + J8].rearrange("p (j e) -> p j e", j=NJ)
            top8 = R[:, 352:352 + J8].rearrange("p (j e) -> p j e", j=NJ)
            s1 = R[:, 384:384 + J8].rearrange("p (j e) -> p j e", j=NJ)
            s12 = R[:, 416:416 + J8].rearrange("p (j e) -> p j e", j=NJ)
            dm = R[:, 448:448 + NJ]
            ex = R[:, 452:452 + NJ]
            p1 = R[:, 456:456 + NJ]
            p2 = R[:, 460:460 + NJ]
            gates = R[:, 464:464 + 2 * NJ].rearrange("p (j k) -> p j k", j=NJ)
            M12f, M1f, M2f, posf = R[:, 480:480 + J32], R[:, 608:608 + J32], R[:, 736:736 + J32], R[:, 864:864 + J32]
            dest = R[:, 992:992 + 2 * NJ].rearrange("p (j k) -> p j k", j=NJ)
            valid = R[:, 1000:1000 + 2 * NJ].rearrange("p (j k) -> p j k", j=NJ)
            gmax_b = gmax.unsqueeze(2).broadcast_to([P, NJ, 4])

            def rdve(fn, extra_r=(), extra_w=()):
                dve(fn, r=[rt] + list(extra_r), w=[rt] + list(extra_w))

            rdve(lambda e: e.tensor_copy(R[:, 0:NJ * 36], PSR.t[:, 0:NJ * 36]), extra_r=[PSR])
            rdve(lambda e: e.tensor_reduce(out=gmax, in_=lg3[:, :, 0:4], axis=AX.X, op=ALU.max))
            rdve(lambda e: e.tensor_tensor(goh, lg3[:, :, 0:4], gmax_b, op=ALU.is_ge))
            rdve(lambda e: e.tensor_tensor(d4, lg3[:, :, 0:4], gmax_b, op=ALU.subtract))
            pool(lambda e: e.tensor_tensor(d4f, econst.t[:, 0:J4], d4f, op=ALU.pow), r=[rt, econst], w=[rt])
            yield
            rdve(lambda e: e.tensor_reduce(out=gsum, in_=d4, axis=AX.X, op=ALU.add))
            rdve(lambda e: e.reciprocal(gprob, gsum))
            rdve(lambda e: e.tensor_tensor(prod, lg3[:, :, 4:36].rearrange("p j (g e) -> p j g e", g=4), goh.unsqueeze(3).broadcast_to([P, NJ, 4, 8]), op=ALU.mult))
            rdve(lambda e: e.tensor_reduce(out=esel, in_=prod.rearrange("p j g e -> p j e g"), axis=AX.X, op=ALU.add))
            for j in range(NJ):
                rdve(lambda e, j=j: e.max(top8[:, j, :], esel[:, j, :]))
            yield
            rdve(lambda e: e.tensor_tensor(s1, esel, top8[:, :, 0:1].broadcast_to([P, NJ, 8]), op=ALU.is_ge))
            rdve(lambda e: e.tensor_tensor(s12, esel, top8[:, :, 1:2].broadcast_to([P, NJ, 8]), op=ALU.is_ge))
            rdve(lambda e: e.tensor_tensor(dm, top8[:, :, 1], top8[:, :, 0], op=ALU.subtract))
            pool(lambda e: e.tensor_tensor(ex, econst.t[:, 0:NJ], dm, op=ALU.pow), r=[rt, econst], w=[rt])
            yield
            rdve(lambda e: e.tensor_scalar(p1, ex, 1.0, None, op0=ALU.add))
            rdve(lambda e: e.reciprocal(p1, p1))
            rdve(lambda e: e.tensor_tensor(p2, ex, p1, op=ALU.mult))
            rdve(lambda e: e.tensor_tensor(gates[:, :, 0], gprob, p1, op=ALU.mult))
            rdve(lambda e: e.tensor_tensor(gates[:, :, 1], gprob, p2, op=ALU.mult))
            goh_b = goh.unsqueeze(3).broadcast_to([P, NJ, 4, 8])
            rdve(lambda e: e.tensor_tensor(M12f.rearrange("p (j g e) -> p j g e", j=NJ, g=4), goh_b, s12.unsqueeze(2).broadcast_to([P, NJ, 4, 8]), op=ALU.mult))
            rdve(lambda e: e.tensor_tensor(M1f.rearrange("p (j g e) -> p j g e", j=NJ, g=4), goh_b, s1.unsqueeze(2).broadcast_to([P, NJ, 4, 8]), op=ALU.mult))
            rdve(lambda e: e.tensor_tensor(M2f, M12f, M1f, op=ALU.subtract))
            yield
            C0 = 192
            for j in range(NJ):
                oj = PSR.t[:, C0 + j * NE:C0 + (j + 1) * NE]
                pe(lambda e, j=j, oj=oj: e.matmul(oj, tri_f.t[:], M12f[:, j * NE:(j + 1) * NE], start=True, stop=False), r=[tri_f, rt], w=[PSR])
                for j2 in range(j):
                    pe(lambda e, j2=j2, oj=oj: e.matmul(oj, ones_mat.t[:], M12f[:, j2 * NE:(j2 + 1) * NE], start=False, stop=False), r=[ones_mat, rt], w=[PSR])
                pe(lambda e, oj=oj: e.matmul(oj, ones_row_f.t[0:1, :], base_row.t[0:1, :], start=False, stop=True), r=[ones_row_f, base_row], w=[PSR])
            pe(lambda e: e.matmul(PSR.t[0:1, 384:384 + J32], ones_col_f.t[:, 0:1], M12f, start=True, stop=True), r=[ones_col_f, rt], w=[PSR])
            cum = PSR.t[:, C0:C0 + J32]
            yield
            rdve(lambda e: e.scalar_tensor_tensor(out=posf, in0=cum, scalar=-1.0, in1=ebaseN.t[:, 0:J32], op0=ALU.add, op1=ALU.add), extra_r=[PSR, ebaseN])
            rdve(lambda e: e.tensor_scalar(prodf, cum, float(CAP), float(NSLOT), op0=ALU.is_gt, op1=ALU.mult), extra_r=[PSR])
            rdve(lambda e: e.tensor_tensor(posf, posf, prodf, op=ALU.add))
            rdve(lambda e: e.tensor_reduce(out=R[0:1, 320:320 + NE], in_=PSR.t[0:1, 384:384 + J32].rearrange("p (j e) -> p e j", j=NJ), axis=AX.X, op=ALU.add), extra_r=[PSR])
            dve(lambda e: e.tensor_tensor(base_row.t[0:1, :], base_row.t[0:1, :], R[0:1, 320:320 + NE], op=ALU.add), r=[rt, base_row], w=[base_row])
            rdve(lambda e: e.tensor_tensor(prodf, M1f, posf, op=ALU.mult))
            rdve(lambda e: e.tensor_reduce(out=dest[:, :, 0], in_=prodf.rearrange("p (j e) -> p j e", j=NJ), axis=AX.X, op=ALU.add))
            rdve(lambda e: e.tensor_tensor(prodf, M2f, posf, op=ALU.mult))
            rdve(lambda e: e.tensor_reduce(out=dest[:, :, 1], in_=prodf.rearrange("p (j e) -> p j e", j=NJ), axis=AX.X, op=ALU.add))
            rdve(lambda e: e.tensor_scalar(valid, dest, float(NSLOT) - 0.5, None, op0=ALU.is_lt))
            dve(lambda e: e.tensor_tensor(tokinfo.t[:, tile0:tile0 + NJ, 2:4], gates, valid, op=ALU.mult), r=[rt], w=[tokinfo])
            dve(lambda e: e.tensor_scalar(tokinfo.t[:, tile0:tile0 + NJ, 0:2], dest, float(NSLOT - 1), None, op0=ALU.min), r=[rt], w=[tokinfo])
            dve(lambda e: e.tensor_copy(tokidx.t[:, tile0:tile0 + NJ, :], tokinfo.t[:, tile0:tile0 + NJ, 0:2]), r=[tokinfo], w=[tokidx])
            dve(lambda e: e.tensor_copy(sidx.t[:, 0:NJ, :], dest), r=[rt], w=[sidx])
            yield
            def emit_scatters():
                for j in range(NJ):
                    for k in range(2):
                        S.dma("pool", scat_sems[j],
                              lambda e, j=j, k=k: e.indirect_dma_start(out=xs_d[:, :], out_offset=bass.IndirectOffsetOnAxis(ap=sidx.t[:, j, k:k + 1], axis=0),
                                                                       in_=h2b_l[j].t[:, :], in_offset=None, bounds_check=bound_reg, oob_is_err=False),
                              r=[h2b_l[j], sidx, xs_zero], w=[], serial=True)
            pending_scatter.append(emit_scatters)

        def pass1():
            with ExitStack() as sc:
                curscope[0] = sc
                wsem = S.newsem()
                ztile = sb("ztile", [P, D], BF16, sc)
                zsem = S.newsem()
                dve(lambda e: e.memset(ztile.t[:], 0.0), w=[ztile])
                zgrp = []
                ZR = 8
                for r_ in range(0, NSLOT, P * ZR):
                    S.dma("act", zsem, lambda e, r_=r_: e.dma_start(out=xs_d[r_:r_ + P * ZR, :].rearrange("(s p) d -> p s d", p=P),
                                                                   in_=ztile.t[:].unsqueeze(1).broadcast_to([P, ZR, D])), r=[ztile], w=[xs_zero], defer=zgrp)
                S.flush(zgrp)
                stgsems = [S.newsem(), S.newsem()]
                w_in = sb("w_in", [P, KD, 2560], BF16, sc)
                w_out = sb("w_out", [P, KD, D], BF16, sc)
                gmix = sb("gmix", [P, KD], F32, sc)
                wsT = sb("wsT", [P, 4, P], BF16, sc)
                wsb_b = sb("wsb_b", [1, 512], BF16, sc)
                alng = sb("alng", [P, 512], F32, sc)
                bconv = sb("bconv", [P, 4, 3], F32, sc)
                tmpsc = ExitStack()
                stg = [sb(f"stg{i}", [P, 2560], F32, tmpsc) for i in range(2)]
                ws_raw = sb("ws_raw", [P, 4, P], F32, tmpsc)
                wsb_f = sb("wsb_f", [1, 512], F32, tmpsc)
                grp = []
                S.dma("sp", wsem, lambda e: e.dma_start(out=gmix.t[:], in_=norm_mix_g[0].rearrange("(k p) -> p k", p=P), allow_slow_non_contiguous=True), w=[gmix], defer=grp)
                S.dma("sp", wsem, lambda e: e.dma_start(out=ws_raw.t[:], in_=a_ws[0].rearrange("h t s -> t h s")), w=[ws_raw], defer=grp)
                S.dma("sp", wsem, lambda e: e.dma_start(out=wsb_f.t[0:1, :], in_=a_ws_b[0].rearrange("(o h) t -> o (h t)", o=1)), w=[wsb_f], defer=grp)
                S.dma("sp", wsem, lambda e: e.dma_start(out=alng.t[:], in_=a_ln_g[0].partition_broadcast(P)), w=[alng], defer=grp)
                for k_ in range(3):
                    S.dma("sp", wsem, lambda e, k_=k_: e.dma_start(out=bconv.t[:, :, k_], in_=b_conv_w[0, k_].rearrange("(c p) -> p c", p=P), allow_slow_non_contiguous=True), w=[bconv], defer=grp)
                S.flush(grp)
                load_cast_rows(stg, stgsems, w_in, lambda kc: ab_w_in[0, kc * P:(kc + 1) * P, :], KD, 2560, gmix)
                load_cast_rows(stg, stgsems, w_out, lambda kc: ab_w_out[0, kc * P:(kc + 1) * P, :], KD, D, None)
                dve(lambda e: e.tensor_copy(wsb_b.t[:], wsb_f.t[:]), r=[wsb_f], w=[wsb_b])
                for h in range(4):
                    pe(lambda e, h=h: e.transpose(ps[5].t[:, h * P:(h + 1) * P], ws_raw.t[:, h, :], ident_f.t[:]), r=[ws_raw, ident_f], w=[ps[5]])
                    dve(lambda e, h=h: e.tensor_tensor(wsT.t[:, h, :], ps[5].t[:, h * P:(h + 1) * P], tri_f.t[:], op=ALU.mult), r=[ps[5], tri_f], w=[wsT])
                S.barrier(skip=[zsem])
                tmpsc.close()
                RW = route_bufs(sc, 0, wsem, MT1 // P)
                junk = RW[5]
                NJ = MT1 // P
                xt_l = [sb(f"xt{i}", [P, NJ, D], F32, sc) for i in range(2)]
                xsem = [S.newsem(), S.newsem()]
                stsem = [S.newsem(), S.newsem()]
                hb_l = [sb(f"hb{i}", [P, D], BF16, sc) for i in range(2)]
                hT = sb("hT", [P, KD, MT1], BF16, sc)
                ss = sb("ss", [P, NJ], F32, sc)
                rstd = sb("rstd", [P, NJ], F32, sc)
                ug = sb("ug", [P, 4, MT1], BF16, sc)
                vg_l = [sb(f"vg{i}", [P, 512], F32, sc) for i in range(2)]
                vn_l = [sb(f"vn{i}", [P, 512], BF16, sc) for i in range(2)]
                st6 = sb("st6", [P, 6], F32, sc)
                mv = sb("mv", [P, 4], F32, sc)
                xb_sb = sb("xb_sb", [P, MT1], F32, sc)
                cx = sb("cx", [P, 4, MT1 + 2], F32, sc)
                cv_l = [sb(f"cv{i}", [P, MT1], F32, sc) for i in range(2)]
                yT_l = [sb(f"yT{i}", [P, KD, MT1], BF16, sc) for i in range(2)]
                pool(lambda e: e.memset(cx.t[:], 0.0), w=[cx])
                zrot = [0]

                def zchunk(col0):
                    pz = ps[zrot[0] % 2]
                    zrot[0] += 1
                    for kc in range(KD):
                        pe(lambda e, kc=kc: e.matmul(pz.t[:, 0:MT1], w_in.t[:, kc, col0:col0 + P], hT.t[:, kc, :], start=(kc == 0), stop=(kc == KD - 1)),
                           r=[w_in, hT], w=[pz])
                    return pz

                NM = N // MT1

                def load1(m):
                    xt = xt_l[m % 2]
                    S.dma("sp", xsem[m % 2], lambda e, m=m, xt=xt: e.dma_start(out=xt.t[:], in_=x_d[m * MT1:(m + 1) * MT1, :].rearrange("(j p) d -> p j d", p=P)), w=[xt])

                def front1(m):
                    xt = xt_l[m % 2]
                    yT = yT_l[m % 2]
                    norm_and_transpose(sc, xt, NJ, hb_l, hT, ss, rstd, junk, ps[3])
                    yield
                    for c in range(4):
                        pz = zchunk(c * P)
                        act(lambda e, c=c, pz=pz: e.activation(out=ug.t[:, c, :], in_=pz.t[:, 0:MT1], func=AF.Gelu_apprx_tanh), r=[pz], w=[ug])
                        yield
                        pass
                    def vmm(j):
                        pv = ps[4] if j % 2 == 0 else ps[3]
                        for kc in range(KD):
                            pe(lambda e, kc=kc, j=j, pv=pv: e.matmul(pv.t[:, 0:512], hT.t[:, kc, j * P:(j + 1) * P], w_in.t[:, kc, 512:1024], start=(kc == 0), stop=(kc == KD - 1)),
                               r=[hT, w_in], w=[pv])

                    vmm(0)
                    for j in range(NJ):
                        vg = vg_l[j % 2]
                        vn = vn_l[j % 2]
                        pv = ps[4] if j % 2 == 0 else ps[3]
                        if j + 1 < NJ:
                            vmm(j + 1)
                        act(lambda e, vg=vg, pv=pv: e.activation(out=vg.t[:], in_=pv.t[:, 0:512], func=AF.Gelu_apprx_tanh, accum_out=st6.t[:, 0:1]), r=[pv], w=[vg, st6])
                        dump("vg", vg.t[:], 512, [vg])
                        act(lambda e, vg=vg: e.activation(out=junk.t[:, 0:512], in_=vg.t[:], func=AF.Square, accum_out=st6.t[:, 1:2]), r=[vg], w=[junk, st6])
                        pool(lambda e: e.tensor_scalar(mv.t[:, 0:1], st6.t[:, 0:1], 1.0 / 512, None, op0=ALU.mult), r=[st6], w=[mv])
                        pool(lambda e: e.tensor_tensor(mv.t[:, 1:2], mv.t[:, 0:1], mv.t[:, 0:1], op=ALU.mult), r=[mv], w=[mv])
                        pool(lambda e: e.tensor_scalar(mv.t[:, 2:3], st6.t[:, 1:2], 1.0 / 512, EPS, op0=ALU.mult, op1=ALU.add), r=[st6, mv], w=[mv])
                        pool(lambda e: e.tensor_tensor(mv.t[:, 2:3], mv.t[:, 2:3], mv.t[:, 1:2], op=ALU.subtract), r=[mv], w=[mv])
                        pool(lambda e: e.tensor_tensor(mv.t[:, 2:3], mv.t[:, 2:3], mhalf.t[:, 0:1], op=ALU.pow), r=[mv, mhalf], w=[mv])
                        dve(lambda e, vg=vg: e.tensor_scalar(vg.t[:], vg.t[:], mv.t[:, 0:1], mv.t[:, 2:3], op0=ALU.subtract, op1=ALU.mult), r=[vg, mv], w=[vg])
                        pool(lambda e, vg=vg, vn=vn: e.tensor_tensor(vn.t[:], vg.t[:], alng.t[:], op=ALU.mult), r=[vg, alng], w=[vn])
                        dump("mv", mv.t[:], 4, [mv])
                        dump("vn", vn.t[:], 512, [vn])
                        for h in range(4):
                            pe(lambda e, h=h, vn=vn: e.matmul(ps[5].t[:, h * P:(h + 1) * P], vn.t[:, h * P:(h + 1) * P], wsT.t[:, h, :], start=True, stop=False),
                               r=[vn, wsT], w=[ps[5]])
                            pe(lambda e, h=h: e.matmul(ps[5].t[:, h * P:(h + 1) * P], ones_row_b.t[0:1, :], wsb_b.t[0:1, h * P:(h + 1) * P], start=False, stop=True),
                               r=[ones_row_b, wsb_b], w=[ps[5]])
                        dve(lambda e, j=j: e.tensor_tensor(yT.t[:, 0:4, j * P:(j + 1) * P], ug.t[:, :, j * P:(j + 1) * P],
                                                           ps[5].t[:].rearrange("p (h t) -> p h t", h=4), op=ALU.mult), r=[ug, ps[5]], w=[yT])
                        yield
                        dump("pm", ps[5].t[:], 512, [ps[5]])
                        dump("yTa", yT.t[:, 0, 0:P], P, [yT])
                    for c in range(4):
                        cv = cv_l[c % 2]
                        pxb = zchunk(2048 + c * P)
                        act(lambda e, pxb=pxb: e.activation(out=xb_sb.t[:], in_=pxb.t[:, 0:MT1], func=AF.Copy), r=[pxb], w=[xb_sb])
                        pgc = zchunk(1536 + c * P)
                        dve(lambda e, c=c, pgc=pgc: e.tensor_tensor(cx.t[:, c, 2:MT1 + 2], pgc.t[:, 0:MT1], xb_sb.t[:], op=ALU.mult), r=[pgc, xb_sb], w=[cx])
                        act(lambda e, c=c, cv=cv: e.activation(out=cv.t[:], in_=cx.t[:, c, 2:MT1 + 2], func=AF.Identity, scale=bconv.t[:, c, 2:3]), r=[cx, bconv], w=[cv])
                        dve(lambda e, c=c, cv=cv: e.scalar_tensor_tensor(out=cv.t[:], in0=cx.t[:, c, 1:MT1 + 1], scalar=bconv.t[:, c, 1:2], in1=cv.t[:], op0=ALU.mult, op1=ALU.add),
                             r=[cx, bconv, cv], w=[cv])
                        dve(lambda e, c=c, cv=cv: e.scalar_tensor_tensor(out=cv.t[:], in0=cx.t[:, c, 0:MT1], scalar=bconv.t[:, c, 0:1], in1=cv.t[:], op0=ALU.mult, op1=ALU.add),
                             r=[cx, bconv, cv], w=[cv])
                        pool(lambda e, c=c: e.tensor_copy(cx.t[:, c, 0:2], cx.t[:, c, MT1:MT1 + 2]), r=[cx], w=[cx])
                        pgb = zchunk(1024 + c * P)
                        dve(lambda e, c=c, pgb=pgb, cv=cv: e.tensor_tensor(yT.t[:, 4 + c, :], pgb.t[:, 0:MT1], cv.t[:], op=ALU.mult), r=[pgb, cv], w=[yT])
                        yield
                        pass
                        pass
                def back1(m):
                    xt = xt_l[m % 2]
                    yT = yT_l[m % 2]
                    for j in range(NJ):
                        tile = m * NJ + j
                        for half in range(2):
                            for c in range(KD):
                                pe(lambda e, c=c, half=half, j=j: e.matmul(psO.t[:, half * 512:(half + 1) * 512], yT.t[:, c, j * P:(j + 1) * P],
                                                                          w_out.t[:, c, half * 512:(half + 1) * 512], start=(c == 0), stop=(c == KD - 1)),
                                   r=[yT, w_out], w=[psO])
                        dve(lambda e, j=j, xt=xt: e.tensor_tensor(xt.t[:, j, :], xt.t[:, j, :], psO.t[:], op=ALU.add), r=[xt, psO], w=[xt])
                        yield
                    yield from route_batch(0, xt, m * NJ, NJ, RW)
                    S.dma("sp", stsem[m % 2], lambda e, m=m, xt=xt: e.dma_start(out=xres_d[m * MT1:(m + 1) * MT1, :].rearrange("(j p) d -> p j d", p=P), in_=xt.t[:]),
                          r=[xt], w=[])

                load1(0)
                if NM > 1:
                    load1(1)
                interleave(front1(0))
                for m in range(NM):
                    interleave(front1(m + 1) if m + 1 < NM else None, back1(m))
                    if m + 2 < NM:
                        load1(m + 2)
                flush_scatters()
                S.barrier()

        def pass_experts(lyr):
            with ExitStack() as sc:
                curscope[0] = sc
                stg = [sb(f"estg{i}", [P, 4096], F32, sc) for i in range(3)]
                stgsem = [S.newsem() for _ in range(3)]
                wg_l = [sb(f"wg{i}", [P, KD, 512], BF16, sc) for i in range(2)]
                wu_l = [sb(f"wu{i}", [P, KD, 512], BF16, sc) for i in range(2)]
                wd_l = [sb(f"wd{i}", [P, 4, D], BF16, sc) for i in range(2)]
                xe_l = [sb(f"xe{i}", [P, 4, D], BF16, sc) for i in range(2)]
                xesem = [S.newsem(), S.newsem()]
                XT = sb("XT", [P, KD, 512], BF16, sc)
                sg_l = [sb(f"sg{i}", [P, 512], F32, sc) for i in range(2)]
                hidT = sb("hidT", [P, 4, 512], BF16, sc)
                ye_l = [sb(f"ye{i}", [P, D], BF16, sc) for i in range(2)]
                yesem = [S.newsem(), S.newsem()]
                psO_h = [Buf("psO_h0"), Buf("psO_h1")]
                blocks = []
                r0 = 0
                while r0 < CAP:
                    nb = min(4, (CAP - r0) // P)
                    blocks.append((r0, nb))
                    r0 += nb * P
                items = [(ex, r0, nb) for ex in range(NE) for (r0, nb) in blocks]
                scnt = [0]

                def emit_w(ex):
                    wg, wu, wd = wg_l[ex % 2], wu_l[ex % 2], wd_l[ex % 2]
                    for wi, (dst, src, nk) in enumerate(((wg, moe_w_gate[lyr, ex].rearrange("(k p) f -> p k f", p=P), KD),
                                                         (wu, moe_w_up[lyr, ex].rearrange("(k p) f -> p k f", p=P), KD),
                                                         (wd, moe_w_down[lyr, ex].rearrange("(k p) f -> p k f", p=P), 4))):
                        st = stg[scnt[0] % 3]
                        sem = stgsem[scnt[0] % 3]
                        scnt[0] += 1
                        S.dma("sp", sem, lambda e, st=st, src=src, nk=nk: e.dma_start(out=st.t[:].rearrange("p (k f) -> p k f", k=nk), in_=src), w=[st])
                        if wi == 0:
                            act(lambda e, st=st, dst=dst, nk=nk: e.activation(out=dst.t[:], in_=st.t[:].rearrange("p (k f) -> p k f", k=nk), func=AF.Copy), r=[st], w=[dst])
                        elif wi == 1:
                            pool(lambda e, st=st, dst=dst, nk=nk: e.tensor_copy(dst.t[:], st.t[:].rearrange("p (k f) -> p k f", k=nk)), r=[st], w=[dst])
                        else:
                            dve(lambda e, st=st, dst=dst, nk=nk: e.tensor_copy(dst.t[:], st.t[:].rearrange("p (k f) -> p k f", k=nk)), r=[st], w=[dst])

                def emit_x(i):
                    ex, r0, nb = items[i]
                    xe = xe_l[i % 2]
                    row0 = ex * CAP + r0
                    S.dma("sp", xesem[i % 2], lambda e: e.dma_start(out=xe.t[:, 0:nb, :], in_=xs_d[row0:row0 + nb * P, :].rearrange("(s p) d -> p s d", p=P)),
                          r=[xs_b], w=[xe])

                XT_l = [XT, sb("XTb", [P, KD, 512], BF16, sc)]
                ycnt = [0]

                def gen_transposes(i):
                    ex, r0, nb = items[i]
                    xe = xe_l[i % 2]
                    XTi = XT_l[i % 2]
                    for s_ in range(nb):
                        ptb = ps[3] if s_ % 2 == 0 else ps[5]
                        ptv = ptb.t[:].bitcast(BF16)
                        for kc in range(KD):
                            pe(lambda e, kc=kc, s_=s_, xe=xe, ptv=ptv: e.transpose(ptv[:, kc * P:(kc + 1) * P], xe.t[:, s_, kc * P:(kc + 1) * P], ident_b.t[:]),
                               r=[xe, ident_b], w=[ptb])
                        if s_ % 2 == 0:
                            dve(lambda e, s_=s_, ptv=ptv: e.tensor_copy(XTi.t[:, :, s_ * P:(s_ + 1) * P], ptv.rearrange("p (k t) -> p k t", k=KD)), r=[ptb], w=[XTi])
                        else:
                            act(lambda e, s_=s_, ptv=ptv: e.activation(out=XTi.t[:, :, s_ * P:(s_ + 1) * P], in_=ptv.rearrange("p (k t) -> p k t", k=KD), func=AF.Copy), r=[ptb], w=[XTi])
                        yield

                def gen_compute(i):
                    ex, r0, nb = items[i]
                    wg, wu, wd = wg_l[ex % 2], wu_l[ex % 2], wd_l[ex % 2]
                    XTi = XT_l[i % 2]
                    row0 = ex * CAP + r0
                    W_ = nb * P
                    for fc in range(4):
                        pg = ps[0] if fc % 2 == 0 else ps[2]
                        pu = ps[1] if fc % 2 == 0 else ps[4]
                        sg = sg_l[fc % 2]
                        for kc in range(KD):
                            pe(lambda e, kc=kc, fc=fc, pg=pg: e.matmul(pg.t[:, 0:W_], wg.t[:, kc, fc * P:(fc + 1) * P], XTi.t[:, kc, 0:W_], start=(kc == 0), stop=(kc == KD - 1)),
                               r=[wg, XTi], w=[pg])
                        for kc in range(KD):
                            pe(lambda e, kc=kc, fc=fc, pu=pu: e.matmul(pu.t[:, 0:W_], wu.t[:, kc, fc * P:(fc + 1) * P], XTi.t[:, kc, 0:W_], start=(kc == 0), stop=(kc == KD - 1)),
                               r=[wu, XTi], w=[pu])
                        act(lambda e, pg=pg, sg=sg: e.activation(out=sg.t[:, 0:W_], in_=pg.t[:, 0:W_], func=AF.Silu), r=[pg], w=[sg])
                        dve(lambda e, fc=fc, pu=pu, sg=sg: e.tensor_tensor(hidT.t[:, fc, 0:W_], sg.t[:, 0:W_], pu.t[:, 0:W_], op=ALU.mult), r=[sg, pu], w=[hidT])
                        yield
                    for s_ in range(nb):
                        ye = ye_l[ycnt[0] % 2]
                        sem = yesem[ycnt[0] % 2]
                        ycnt[0] += 1
                        for half in range(2):
                            hb_ = psO_h[half]
                            for fc in range(4):
                                pe(lambda e, fc=fc, half=half, s_=s_: e.matmul(psO.t[:, half * 512:(half + 1) * 512], hidT.t[:, fc, s_ * P:(s_ + 1) * P],
                                                                             wd.t[:, fc, half * 512:(half + 1) * 512], start=(fc == 0), stop=(fc == 3)),
                                   r=[hidT, wd], w=[hb_])
                            if half == 0:
                                act(lambda e, ye=ye: e.activation(out=ye.t[:, 0:512], in_=psO.t[:, 0:512], func=AF.Copy), r=[hb_], w=[ye])
                            else:
                                dve(lambda e, ye=ye: e.tensor_copy(ye.t[:, 512:1024], psO.t[:, 512:1024]), r=[hb_], w=[ye])
                        S.dma("act", sem, lambda e, ye=ye, row0=row0, s_=s_: e.dma_start(out=ys_d[row0 + s_ * P:row0 + (s_ + 1) * P, :], in_=ye.t[:]), r=[ye], w=[])
                        yield

                emit_w(0)
                emit_x(0)
                if len(items) > 1:
                    emit_x(1)
                interleave(gen_transposes(0))
                for i, (ex, r0, nb) in enumerate(items):
                    if r0 == 0 and ex + 1 < NE:
                        emit_w(ex + 1)
                    interleave(gen_compute(i), gen_transposes(i + 1) if i + 1 < len(items) else None)
                    if i + 2 < len(items):
                        emit_x(i + 2)
                S.barrier()

        def combine(xt, j, tile, yg_l, ygsem, cnt):
            yg = yg_l[cnt % 2]
            sem = ygsem[cnt % 2]
            for k in range(2):
                S.dma("pool", sem, lambda e, k=k, yg=yg: e.indirect_dma_start(out=yg.t[:, k, :], out_offset=None, in_=ys_d[:, :],
                                                                               in_offset=bass.IndirectOffsetOnAxis(ap=tokidx.t[:, tile, k:k + 1], axis=0)),
                      r=[ys_b, tokidx], w=[yg], serial=True)
            for k in range(2):
                dve(lambda e, k=k, yg=yg: e.scalar_tensor_tensor(out=xt.t[:, j, :], in0=yg.t[:, k, :], scalar=tokinfo.t[:, tile, 2 + k:3 + k], in1=xt.t[:, j, :],
                                                                 op0=ALU.mult, op1=ALU.add), r=[yg, tokinfo, xt], w=[xt])

        def pass3():
            with ExitStack() as sc:
                curscope[0] = sc
                wsem = S.newsem()
                stgsems = [S.newsem(), S.newsem()]
                w_in = sb("cw_in", [P, KD, 2048], BF16, sc)
                w_out = sb("cw_out", [P, KD, D], BF16, sc)
                gmix = sb("gmix1", [P, KD], F32, sc)
                wa = sb("wa", [P, 8, P], BF16, sc)
                wx = sb("wx", [P, 8, P], BF16, sc)
                cw = sb("cw", [P, KD, 4], F32, sc)
                cvec = sb("cvec", [P, 8, KD], F32, sc)
                tmpsc = ExitStack()
                stg = [sb(f"stg3{i}", [P, 2048], F32, tmpsc) for i in range(2)]
                wa_f = sb("wa_f", [P, 8, P], F32, tmpsc)
                wx_f = sb("wx_f", [P, 8, P], F32, tmpsc)
                grp = []
                S.dma("sp", wsem, lambda e: e.dma_start(out=gmix.t[:], in_=norm_mix_g[1].rearrange("(k p) -> p k", p=P), allow_slow_non_contiguous=True), w=[gmix], defer=grp)
                S.dma("sp", wsem, lambda e: e.dma_start(out=wa_f.t[:], in_=c_w_a[0].rearrange("h i j -> i h j")), w=[wa_f], defer=grp)
                S.dma("sp", wsem, lambda e: e.dma_start(out=wx_f.t[:], in_=c_w_x[0].rearrange("h i j -> i h j")), w=[wx_f], defer=grp)
                for k_ in range(4):
                    S.dma("sp", wsem, lambda e, k_=k_: e.dma_start(out=cw.t[:, :, k_], in_=c_conv_w[0, k_].rearrange("(c p) -> p c", p=P), allow_slow_non_contiguous=True), w=[cw], defer=grp)
                for i_, src in enumerate((c_conv_b, c_b_a, c_b_x, c_lambda)):
                    S.dma("sp", wsem, lambda e, i_=i_, src=src: e.dma_start(out=cvec.t[:, i_, :], in_=src[0].rearrange("(c p) -> p c", p=P), allow_slow_non_contiguous=True), w=[cvec], defer=grp)
                S.flush(grp)
                dve(lambda e: e.tensor_copy(wa.t[:], wa_f.t[:]), r=[wa_f], w=[wa])
                dve(lambda e: e.tensor_copy(wx.t[:], wx_f.t[:]), r=[wx_f], w=[wx])
                act(lambda e: e.activation(out=cvec.t[:, 4, :], in_=cvec.t[:, 3, :], func=AF.Exp, scale=-1.0), r=[cvec], w=[cvec])
                dve(lambda e: e.tensor_scalar(cvec.t[:, 6, :], cvec.t[:, 4, :], 1.0 / 3.0, -0.5, op0=ALU.mult, op1=ALU.add), r=[cvec], w=[cvec])
                dve(lambda e: e.tensor_tensor(cvec.t[:, 6, :], cvec.t[:, 6, :], cvec.t[:, 4, :], op=ALU.mult), r=[cvec], w=[cvec])
                dve(lambda e: e.tensor_scalar(cvec.t[:, 6, :], cvec.t[:, 6, :], 1.0, None, op0=ALU.add), r=[cvec], w=[cvec])
                dve(lambda e: e.tensor_tensor(cvec.t[:, 6, :], cvec.t[:, 6, :], cvec.t[:, 4, :], op=ALU.mult), r=[cvec], w=[cvec])
                dve(lambda e: e.tensor_scalar(cvec.t[:, 4, :], cvec.t[:, 6, :], -8.0, None, op0=ALU.mult), r=[cvec], w=[cvec])
                dve(lambda e: e.tensor_scalar(cvec.t[:, 5, :], cvec.t[:, 6, :], -16.0, None, op0=ALU.mult), r=[cvec], w=[cvec])
                load_cast_rows(stg, stgsems, w_in, lambda kc: c_w_in[0, kc * P:(kc + 1) * P, :], KD, 2048, gmix)
                load_cast_rows(stg, stgsems, w_out, lambda kc: c_w_out[0, kc * P:(kc + 1) * P, :], KD, D, None)
                S.barrier()
                tmpsc.close()
                RW = route_bufs(sc, 1, wsem, MT3 // P)
                junk = RW[5]
                dve(lambda e: e.tensor_scalar(cvec.t[:, 6, :], cvec.t[:, 1, :], 0.5, None, op0=ALU.mult), r=[cvec], w=[cvec])
                dve(lambda e: e.tensor_scalar(cvec.t[:, 7, :], cvec.t[:, 2, :], 0.5, None, op0=ALU.mult), r=[cvec], w=[cvec])
                dve(lambda e: e.tensor_scalar(cvec.t[:, 3, :], cvec.t[:, 4, :], 0.5, None, op0=ALU.mult), r=[cvec], w=[cvec])
                NJ = MT3 // P
                NM = N // MT3
                xt_l = [sb(f"xt3{i}", [P, NJ, D], F32, sc) for i in range(3)]
                xsem = [S.newsem() for _ in range(3)]
                stsem = [S.newsem() for _ in range(3)]
                yg_l = [sb(f"yg{i}", [P, 2, D], BF16, sc) for i in range(4)]
                ygsem = [S.newsem() for _ in range(4)]
                hb_l = [sb(f"hb3{i}", [P, D], BF16, sc) for i in range(2)]
                hT = sb("hT3", [P, KD, MT3], BF16, sc)
                ss = sb("ss3", [P, NJ], F32, sc)
                rstd = sb("rstd3", [P, NJ], F32, sc)
                gg_l = [sb(f"gg{i}", [P, KD, MT3], BF16, sc) for i in range(2)]
                xr = sb("xr", [P, KD, MT3 + 3], F32, sc)
                xcf_l = [sb(f"xcf{i}", [P, MT3], F32, sc) for i in range(2)]
                XCB_l = [sb(f"XCB{i}", [P, KD, MT3], BF16, sc) for i in range(2)]
                TR = sb("TR", [P, KD, MT3], F32, sc)
                TI = sb("TI", [P, KD, MT3], F32, sc)
                AA = sb("AA", [P, KD, MT3], F32, sc)
                A2 = sb("A2", [P, KD, MT3], F32, sc)
                HS = A2
                hstate = sb("hstate", [P, KD], F32, sc)
                yT_l = [sb(f"yT3{i}", [P, KD, MT3], BF16, sc) for i in range(2)]
                pool(lambda e: e.memset(xr.t[:], 0.0), w=[xr])
                pool(lambda e: e.memset(hstate.t[:], 0.0), w=[hstate])

                def cb(nm):
                    return [Buf(f"{nm}{c}") for c in range(KD)]
                xr_c, TR_c, TI_c, AA_c, A2_c = cb("xr"), cb("TR"), cb("TI"), cb("AA"), cb("A2")
                gg_c = [cb("gga"), cb("ggb")]
                XCB_c = [cb("XCBa"), cb("XCBb")]
                yT_c = [cb("yTa"), cb("yTb")]
                S.barrier()
                zrot = [0]

                def zchunk(col0):
                    pz = ps[zrot[0] % 2]
                    zrot[0] += 1
                    for kc in range(KD):
                        pe(lambda e, kc=kc: e.matmul(pz.t[:, 0:MT3], w_in.t[:, kc, col0:col0 + P], hT.t[:, kc, :], start=(kc == 0), stop=(kc == KD - 1)),
                           r=[w_in, hT], w=[pz])
                    return pz

                def emit_load(m):
                    xt = xt_l[m % 3]
                    S.dma("sp", xsem[m % 3], lambda e: e.dma_start(out=xt.t[:], in_=xres_d[m * MT3:(m + 1) * MT3, :].rearrange("(j p) d -> p j d", p=P)),
                          r=[xres_b], w=[xt])
                    for j in range(NJ):
                        tile = m * NJ + j
                        yg = yg_l[tile % 4]
                        for k in range(2):
                            S.dma("pool", ygsem[tile % 4], lambda e, k=k, yg=yg, tile=tile: e.indirect_dma_start(
                                out=yg.t[:, k, :], out_offset=None, in_=ys_d[:, :],
                                in_offset=bass.IndirectOffsetOnAxis(ap=tokidx.t[:, tile, k:k + 1], axis=0)), r=[ys_b, tokidx], w=[yg], serial=True)

                def stageH(m):
                    xt = xt_l[m % 3]
                    sl = m % 2
                    gg, XCB = gg_l[sl], XCB_l[sl]
                    for j in range(NJ):
                        tile = m * NJ + j
                        yg = yg_l[tile % 4]
                        for k in range(2):
                            dve(lambda e, k=k, yg=yg, j=j, tile=tile: e.scalar_tensor_tensor(out=xt.t[:, j, :], in0=yg.t[:, k, :], scalar=tokinfo.t[:, tile, 2 + k:3 + k],
                                                                                         in1=xt.t[:, j, :], op0=ALU.mult, op1=ALU.add), r=[yg, tokinfo, xt], w=[xt])
                    yield
                    norm_and_transpose(sc, xt, NJ, hb_l, hT, ss, rstd, junk, ps[3])
                    yield
                    for c in range(KD):
                        pz = zchunk(c * P)
                        act(lambda e, c=c, pz=pz: e.activation(out=gg.t[:, c, :], in_=pz.t[:, 0:MT3], func=AF.Gelu_apprx_tanh), r=[pz], w=[gg_c[sl][c]])
                        if c % 2 == 1:
                            yield
                    for c in range(KD):
                        pz = zchunk(D + c * P)
                        act(lambda e, c=c, pz=pz: e.activation(out=xr.t[:, c, 3:MT3 + 3], in_=pz.t[:, 0:MT3], func=AF.Copy), r=[pz], w=[xr_c[c]])
                        if c % 2 == 1:
                            yield
                    for c in range(KD):
                        xcf = xcf_l[c % 2]
                        act(lambda e, c=c, xcf=xcf: e.activation(out=xcf.t[:], in_=xr.t[:, c, 3:MT3 + 3], func=AF.Identity, scale=cw.t[:, c, 3:4], bias=cvec.t[:, 0, c:c + 1]),
                            r=[xr_c[c], cw, cvec], w=[xcf])
                        for k in range(2):
                            dve(lambda e, c=c, k=k, xcf=xcf: e.scalar_tensor_tensor(out=xcf.t[:], in0=xr.t[:, c, k:MT3 + k], scalar=cw.t[:, c, k:k + 1], in1=xcf.t[:],
                                                                                  op0=ALU.mult, op1=ALU.add), r=[xr_c[c], cw, xcf], w=[xcf])
                        dve(lambda e, c=c, xcf=xcf: e.scalar_tensor_tensor(out=XCB.t[:, c, :], in0=xr.t[:, c, 2:MT3 + 2], scalar=cw.t[:, c, 2:3], in1=xcf.t[:],
                                                                          op0=ALU.mult, op1=ALU.add), r=[xr_c[c], cw, xcf], w=[XCB_c[sl][c]])
                        if c % 2 == 1:
                            yield
                    pool(lambda e: e.tensor_copy(xr.t[:, :, 0:3], xr.t[:, :, MT3:MT3 + 3]), r=xr_c, w=xr_c)
                    yield

                def stageE(m):
                    sl = m % 2
                    gg, XCB, yT = gg_l[sl], XCB_l[sl], yT_l[sl]
                    for c in range(KD):
                        pr, pi = ps[4], ps[5]
                        pe(lambda e, c=c, pr=pr: e.matmul(pr.t[:, 0:MT3], wa.t[:, c, :], XCB.t[:, c, :], start=True, stop=True), r=[wa, XCB_c[sl][c]], w=[pr])
                        pe(lambda e, c=c, pi=pi: e.matmul(pi.t[:, 0:MT3], wx.t[:, c, :], XCB.t[:, c, :], start=True, stop=True), r=[wx, XCB_c[sl][c]], w=[pi])
                        act(lambda e, c=c, pr=pr: e.activation(out=TR.t[:, c, :], in_=pr.t[:, 0:MT3], func=AF.Tanh, scale=0.5, bias=cvec.t[:, 6, c:c + 1]), r=[pr, cvec], w=[TR_c[c]])
                        act(lambda e, c=c, pi=pi: e.activation(out=TI.t[:, c, :], in_=pi.t[:, 0:MT3], func=AF.Tanh, scale=0.5, bias=cvec.t[:, 7, c:c + 1]), r=[pi, cvec], w=[TI_c[c]])
                        if c % 2 == 1:
                            yield
                    for c in range(KD):
                        act(lambda e, c=c: e.activation(out=AA.t[:, c, :], in_=TR.t[:, c, :], func=AF.Exp, scale=cvec.t[:, 3, c:c + 1], bias=cvec.t[:, 3, c:c + 1]), r=[TR_c[c], cvec], w=[AA_c[c]])
                        act(lambda e, c=c: e.activation(out=A2.t[:, c, :], in_=TR.t[:, c, :], func=AF.Exp, scale=cvec.t[:, 4, c:c + 1], bias=cvec.t[:, 4, c:c + 1]), r=[TR_c[c], cvec], w=[A2_c[c]])
                        if c % 4 == 3:
                            yield
                    for c in range(KD):
                        dve(lambda e, c=c: e.scalar_tensor_tensor(out=TI.t[:, c, :], in0=TI.t[:, c, :], scalar=1.0, in1=XCB.t[:, c, :], op0=ALU.add, op1=ALU.mult),
                            r=[TI_c[c], XCB_c[sl][c]], w=[TI_c[c]])
                    yield
                    for c in range(KD):
                        act(lambda e, c=c: e.activation(out=A2.t[:, c, :], in_=A2.t[:, c, :], func=AF.Relu, scale=-1.0, bias=1.0), r=[A2_c[c]], w=[A2_c[c]])
                    yield
                    for c in range(KD):
                        act(lambda e, c=c: e.activation(out=A2.t[:, c, :], in_=A2.t[:, c, :], func=AF.Sqrt), r=[A2_c[c]], w=[A2_c[c]])
                    yield
                    for c in range(KD):
                        dve(lambda e, c=c: e.scalar_tensor_tensor(out=TR.t[:, c, :], in0=A2.t[:, c, :], scalar=0.5, in1=TI.t[:, c, :], op0=ALU.mult, op1=ALU.mult),
                            r=[A2_c[c], TI_c[c]], w=[TR_c[c]])
                    yield
                    for c in range(KD):
                        dve(lambda e, c=c: e.tensor_tensor_scan(HS.t[:, c, :], AA.t[:, c, :], TR.t[:, c, :], hstate.t[:, c:c + 1], op0=ALU.mult, op1=ALU.add),
                            r=[AA_c[c], TR_c[c], hstate], w=[A2_c[c]])
                        if c % 4 == 3:
                            yield
                    dve(lambda e: e.tensor_copy(hstate.t[:, :], HS.t[:, :, MT3 - 1]), r=A2_c, w=[hstate])
                    for c in range(KD):
                        dve(lambda e, c=c: e.tensor_tensor(yT.t[:, c, :], gg.t[:, c, :], HS.t[:, c, :], op=ALU.mult), r=[gg_c[sl][c], A2_c[c]], w=[yT_c[sl][c]])
                    yield

                def stageB(m):
                    xt = xt_l[m % 3]
                    sl = m % 2
                    yT = yT_l[sl]
                    for j in range(NJ):
                        for half in range(2):
                            for c in range(KD):
                                pe(lambda e, c=c, half=half, j=j: e.matmul(psO.t[:, half * 512:(half + 1) * 512], yT.t[:, c, j * P:(j + 1) * P],
                                                                          w_out.t[:, c, half * 512:(half + 1) * 512], start=(c == 0), stop=(c == KD - 1)),
                                   r=[yT_c[sl][c], w_out], w=[psO])
                        dve(lambda e, j=j, xt=xt: e.tensor_tensor(xt.t[:, j, :], xt.t[:, j, :], psO.t[:], op=ALU.add), r=[xt, psO], w=[xt])
                        yield
                    yield from route_batch(1, xt, m * NJ, NJ, RW)
                    S.dma("sp", stsem[m % 3], lambda e, m=m, xt=xt: e.dma_start(out=xres_d[m * MT3:(m + 1) * MT3, :].rearrange("(j p) d -> p j d", p=P), in_=xt.t[:]),
                          r=[xt], w=[])
                    if m + 3 < NM:
                        emit_load(m + 3)

                emit_load(0)
                if NM > 1:
                    emit_load(1)
                for k in range(NM + 2):
                    interleave(stageH(k) if k < NM else None,
                               stageE(k - 1) if 0 <= k - 1 < NM else None,
                               stageB(k - 2) if 0 <= k - 2 < NM else None)
                    if k == 0 and NM > 2:
                        emit_load(2)
                flush_scatters()
                S.barrier()

        def pass5(src_d, do_combine, do_norm):
            with ExitStack() as sc:
                curscope[0] = sc
                NJ = 4
                MT = NJ * P
                NM = N // MT
                xt_l = [sb(f"xt5{i}", [P, NJ, D], F32, sc) for i in range(2)]
                xsem = [S.newsem(), S.newsem()]
                stsem = [S.newsem(), S.newsem()]
                yg_l = [sb(f"yg5{i}", [P, 2, D], BF16, sc) for i in range(8)]
                ygsem = [S.newsem() for _ in range(8)]
                junk = sb("junk5", [P, D], BF16, sc)
                ss = sb("ss5", [P, NJ], F32, sc)
                rstd = sb("rstd5", [P, NJ], F32, sc)

                def load5(m):
                    xt = xt_l[m % 2]
                    S.dma("sp", xsem[m % 2], lambda e: e.dma_start(out=xt.t[:], in_=src_d[m * MT:(m + 1) * MT, :].rearrange("(j p) d -> p j d", p=P)),
                          r=[xres_b], w=[xt])
                    if do_combine:
                        for j in range(NJ):
                            tile = m * NJ + j
                            yg = yg_l[tile % 8]
                            for k in range(2):
                                S.dma("pool", ygsem[tile % 8], lambda e, k=k, yg=yg, tile=tile: e.indirect_dma_start(
                                    out=yg.t[:, k, :], out_offset=None, in_=ys_d[:, :],
                                    in_offset=bass.IndirectOffsetOnAxis(ap=tokidx.t[:, tile, k:k + 1], axis=0)), r=[ys_b, tokidx], w=[yg], serial=True)

                ss_l = [ss, sb("ss5b", [P, NJ], F32, sc)]
                rstd_l = [rstd, sb("rstd5b", [P, NJ], F32, sc)]

                def s1(m):
                    xt = xt_l[m % 2]
                    ssm, rstdm = ss_l[m % 2], rstd_l[m % 2]
                    for j in range(NJ):
                        tile = m * NJ + j
                        if do_combine:
                            yg = yg_l[tile % 8]
                            for k in range(2):
                                dve(lambda e, k=k, yg=yg, j=j, tile=tile: e.scalar_tensor_tensor(out=xt.t[:, j, :], in0=yg.t[:, k, :], scalar=tokinfo.t[:, tile, 2 + k:3 + k],
                                                                                             in1=xt.t[:, j, :], op0=ALU.mult, op1=ALU.add), r=[yg, tokinfo, xt], w=[xt])
                        if do_norm:
                            act(lambda e, j=j, xt=xt: e.activation(out=junk.t[:], in_=xt.t[:, j, :], func=AF.Square, accum_out=ssm.t[:, j:j + 1]), r=[xt], w=[junk, ssm])
                        yield
                    if do_norm:
                        rstd_from_ss(rstdm, ssm, D, NJ)
                    yield

                def s2(m):
                    xt = xt_l[m % 2]
                    rstdm = rstd_l[m % 2]
                    if do_norm:
                        for j in range(NJ):
                            dve(lambda e, j=j, xt=xt: e.scalar_tensor_tensor(out=xt.t[:, j, :], in0=xt.t[:, j, :], scalar=rstdm.t[:, j:j + 1], in1=gfin_rep.t[:],
                                                                             op0=ALU.mult, op1=ALU.mult), r=[xt, rstdm, gfin_rep], w=[xt])
                            yield
                    S.dma("sp", stsem[m % 2], lambda e, m=m, xt=xt: e.dma_start(out=out_d[m * MT:(m + 1) * MT, :].rearrange("(j p) d -> p j d", p=P), in_=xt.t[:]),
                          r=[xt], w=[])
                    if m + 2 < NM:
                        load5(m + 2)
                    yield

                load5(0)
                if NM > 1:
                    load5(1)
                interleave(s1(0))
                for m in range(NM):
                    interleave(s1(m + 1) if m + 1 < NM else None, s2(m))
                S.dma("sp", stsem[0], lambda e: e.dma_start(out=dbg_d, in_=tokinfo.t[:].rearrange("p t f -> p (t f)")), r=[tokinfo], w=[])
                S.barrier()

        pass1()
        if stop_after >= 2:
            pass_experts(0)
        if stop_after >= 3:
            pass3()
        if stop_after >= 4:
            pass_experts(1)
        if stop_after >= 5:
            pass5(xres_d, True, True)
        elif stop_after in (2, 4):
            pass5(xres_d, True, False)
        else:
            pass5(xres_d, False, False)
    global LAST_DUMPS
    LAST_DUMPS = dumps
    return nc


WKEYS = ["norm_mix_g", "norm_ffn_g", "norm_final_g", "ab_w_in", "a_ln_g", "a_ws", "a_ws_b", "b_conv_w", "ab_w_out",
         "c_w_in", "c_conv_w", "c_conv_b", "c_w_a", "c_b_a", "c_w_x", "c_b_x", "c_lambda", "c_w_out",
         "moe_w_rg", "moe_b_rg", "moe_w_re", "moe_b_re", "moe_w_gate", "moe_w_up", "moe_w_down"]

CAP_FULL = 1024


def kernel(**inputs):
    x = np.ascontiguousarray(inputs["x"], dtype=np.float32)
    B, T, _ = x.shape
    nc = build(T, CAP_FULL)
    w = {k: np.ascontiguousarray(inputs[k], dtype=np.float32) for k in WKEYS}
    in_maps = []
    for b in range(B):
        m = dict(w)
        m["x"] = x[b]
        in_maps.append(m)
    res = run_bass_kernel_spmd(nc, in_maps, core_ids=list(range(B)))
    return np.stack([np.asarray(r["out"], dtype=np.float32) for r in res.results], axis=0)
```
